# Optimizing a Trainium2 kernel written in Bass

```python
import math
import jax
import jax.numpy as jnp
from jax import lax
import numpy as np


D_MODEL = 1024
BATCH = 2
SEQ = 8192
DEPTH = 1

GRID_W = 64
CTX_LEN = 256
EPS = 1e-6
N_MOD = 6

D_HY = 512
HY_ORDER = 2
HY_BANDS = 16
HY_EMB = 1 + 2 * HY_BANDS
HY_FFN = 64
HY_DECAY_TARGET = 1e-2
HY_FAST_PCT = 0.3
HY_SLOW_PCT = 1.5

NA_HEADS = 8
HEAD_DIM = 64
D_NA = NA_HEADS * HEAD_DIM
WIN_ROWS = 8
WIN_COLS = 16
ROPE_THETA = 10000.0
NEG_INF = -1e30

PEER_HEADS = 8
PEER_N_KEYS = 128
PEER_N_EXPERTS = PEER_N_KEYS * PEER_N_KEYS
PEER_TOPK = 16
PEER_D_KEY = 256
PEER_D_HALF = PEER_D_KEY // 2
PEER_BLOCK = 128

COL_HY = 0
COL_Q = COL_HY + 3 * D_HY
COL_K = COL_Q + D_NA
COL_V = COL_K + D_NA
COL_G_HY = COL_V + D_NA
COL_G_NA = COL_G_HY + D_MODEL
N_PROJ = COL_G_NA + D_MODEL

kernel_name = 'hybrid_hyena_natten_peer_block'


def rmsnorm(x, g):
    x32 = x.astype(jnp.float32)
    y = x32 * lax.rsqrt(jnp.mean(x32 * x32, axis=-1, keepdims=True) + EPS)
    return (y * g.astype(jnp.float32)).astype(x.dtype)


def modulate(x, g, shift, scale):
    return rmsnorm(x, g) * (1.0 + scale) + shift


def adaln(cond, p):
    m = jax.nn.silu(cond) @ p['w_ada'] + p['b_ada']
    return m.reshape(cond.shape[:-1] + (N_MOD, D_MODEL))


def hyena_filters(L, p):
    f32 = jnp.float32
    t = jnp.linspace(0.0, 1.0, L, dtype=f32)[:, None]
    w = (2.0 * math.pi / L) * jnp.arange(L, dtype=f32)[:, None]
    bands = jnp.linspace(1e-4, HY_BANDS - 1.0, HY_BANDS, dtype=f32)[None, :]
    feats = jnp.concatenate([t, jnp.cos(bands * w), -jnp.sin(bands * w)], axis=-1)
    freq = p['hy_sin_freq'].astype(f32)
    h = jnp.sin(freq * (feats @ p['hy_f1_w'].astype(f32) + p['hy_f1_b'].astype(f32)))
    h = jnp.sin(freq * (h @ p['hy_f2_w'].astype(f32) + p['hy_f2_b'].astype(f32)))
    h = jnp.sin(freq * (h @ p['hy_f3_w'].astype(f32) + p['hy_f3_b'].astype(f32)))
    h = (h @ p['hy_f4_w'].astype(f32)).reshape(L, HY_ORDER, 2, D_HY)
    deltas = jnp.abs(jnp.linspace(math.log(HY_DECAY_TARGET) / HY_SLOW_PCT,
                                  math.log(HY_DECAY_TARGET) / HY_FAST_PCT, D_HY, dtype=f32))
    h = h * jnp.exp(-t * deltas)[:, None, None, :]
    h = h / jnp.sum(jnp.abs(h), axis=(0, 2), keepdims=True)
    fwd, bwd = h[:, :, 0], h[:, :, 1]
    return jnp.concatenate([fwd, jnp.zeros_like(fwd[:1]), bwd[:0:-1]], axis=0)


def hyena(zh, p):
    B, L, _ = zh.shape
    cw = p['hy_conv_w']
    zp = jnp.pad(zh, ((0, 0), (1, 1), (0, 0)))
    zc = cw[0] * zp[:, :-2] + cw[1] * zp[:, 1:-1] + cw[2] * zp[:, 2:] + p['hy_conv_b']
    v, x1, x2 = jnp.split(zc.astype(jnp.float32), 3, axis=-1)
    kf = jnp.fft.rfft(hyena_filters(L, p), axis=0)
    skip = p['hy_skip'].astype(jnp.float32)
    y = v
    for o, gate in enumerate((x1, x2)):
        yf = jnp.fft.rfft(y, n=2 * L, axis=1)
        conv = jnp.fft.irfft(yf * kf[:, o][None], n=2 * L, axis=1)[:, :L]
        y = gate * (conv + skip[o] * y)
    return y.astype(zh.dtype)


def axial_rope(x):
    f32 = jnp.float32
    S = x.shape[1]
    pos = jnp.arange(S, dtype=jnp.int32)
    rows = (pos // GRID_W).astype(f32)
    cols = (pos % GRID_W).astype(f32)
    half = HEAD_DIM // 2
    nf = half // 2
    inv = ROPE_THETA ** (-jnp.arange(nf, dtype=f32) / nf)

    def rot(xp, ps):
        ang = ps[:, None] * inv[None, :]
        cos = jnp.cos(ang)[None, :, None, :]
        sin = jnp.sin(ang)[None, :, None, :]
        a, b = xp[..., :nf], xp[..., nf:]
        return jnp.concatenate([a * cos - b * sin, a * sin + b * cos], axis=-1)

    x32 = x.astype(f32)
    return jnp.concatenate([rot(x32[..., :half], rows), rot(x32[..., half:], cols)], axis=-1).astype(x.dtype)


def neighbourhood_attention(q, k, v, k_ctx, v_ctx, rpb):
    B, S, H, Dh = q.shape
    rows = S // GRID_W
    wr = min(WIN_ROWS, rows)
    r = np.arange(rows)
    row_start = np.clip(r - wr // 2, 0, rows - wr)
    row_idx = row_start[:, None] + np.arange(wr)[None, :]
    col = np.arange(GRID_W)
    col_start = np.clip(col - WIN_COLS // 2, 0, GRID_W - WIN_COLS)
    col_mask = (col[None, :] >= col_start[:, None]) & (col[None, :] < col_start[:, None] + WIN_COLS)
    dr_idx = row_idx - r[:, None] + (WIN_ROWS - 1)
    dc_idx = np.clip(col[None, :] - col[:, None], -(WIN_COLS - 1), WIN_COLS - 1) + (WIN_COLS - 1)
    bias = rpb[:, dr_idx[:, None, :, None], dc_idx[None, :, None, :]]

    qg = q.reshape(B, rows, GRID_W, H, Dh)
    kg = k.reshape(B, rows, GRID_W, H, Dh)[:, row_idx]
    vg = v.reshape(B, rows, GRID_W, H, Dh)[:, row_idx]
    scale = HEAD_DIM ** -0.5
    s_loc = jnp.einsum('brqhd,brikhd->bhrqik', qg, kg).astype(jnp.float32) * scale + bias.astype(jnp.float32)
    s_loc = jnp.where(col_mask[:, None, :], s_loc, NEG_INF).reshape(B, H, rows, GRID_W, wr * GRID_W)
    s_ctx = jnp.einsum('brqhd,bchd->bhrqc', qg, k_ctx).astype(jnp.float32) * scale
    p = jax.nn.softmax(jnp.concatenate([s_loc, s_ctx], axis=-1), axis=-1).astype(v.dtype)
    n_loc = wr * GRID_W
    p_loc = p[..., :n_loc].reshape(B, H, rows, GRID_W, wr, GRID_W)
    out = (jnp.einsum('bhrqik,brikhd->brqhd', p_loc, vg)
           + jnp.einsum('bhrqc,bchd->brqhd', p[..., n_loc:], v_ctx))
    return out.reshape(B, S, H, Dh)


def context_attention(q, k, v):
    s = jnp.einsum('bqhd,bkhd->bhqk', q, k).astype(jnp.float32) * (HEAD_DIM ** -0.5)
    p = jax.nn.softmax(s, axis=-1).astype(v.dtype)
    return jnp.einsum('bhqk,bkhd->bqhd', p, v)


def context_kv(h_ctx, p):
    B, C, _ = h_ctx.shape
    k = (h_ctx @ p['w_in'][:, COL_K:COL_V] + p['b_in'][COL_K:COL_V]).reshape(B, C, NA_HEADS, HEAD_DIM)
    v = (h_ctx @ p['w_in'][:, COL_V:COL_G_HY] + p['b_in'][COL_V:COL_G_HY]).reshape(B, C, NA_HEADS, HEAD_DIM)
    return rmsnorm(k, p['k_norm_g']), v


def token_mixer(h, p, attend):
    B, L, _ = h.shape
    z = h @ p['w_in'] + p['b_in']
    q = rmsnorm(z[..., COL_Q:COL_K].reshape(B, L, NA_HEADS, HEAD_DIM), p['q_norm_g'])
    k = rmsnorm(z[..., COL_K:COL_V].reshape(B, L, NA_HEADS, HEAD_DIM), p['k_norm_g'])
    v = z[..., COL_V:COL_G_HY].reshape(B, L, NA_HEADS, HEAD_DIM)
    y_hy = hyena(z[..., COL_HY:COL_Q], p) @ p['w_hy_out']
    y_na = attend(q, k, v).reshape(B, L, D_NA) @ p['w_na_out']
    gate = jax.nn.sigmoid(z[..., COL_G_HY:N_PROJ].astype(jnp.float32)).astype(h.dtype)
    merged = gate[..., :D_MODEL] * y_hy + gate[..., D_MODEL:] * y_na
    return merged @ p['w_out']


def peer(h, p):
    B, L, D = h.shape
    T = B * L
    hf = h.reshape(T, D)
    q = (hf @ p['peer_w_q']).reshape(T, PEER_HEADS, 2, PEER_D_HALF)
    s = jnp.einsum('thpd,hpnd->thpn', q, p['peer_keys']).astype(jnp.float32)
    top_s, top_i = lax.top_k(s, PEER_TOPK)
    cand_s = (top_s[:, :, 0, :, None] + top_s[:, :, 1, None, :]).reshape(T, PEER_HEADS, PEER_TOPK * PEER_TOPK)
    cand_i = (top_i[:, :, 0, :, None] * PEER_N_KEYS + top_i[:, :, 1, None, :]).reshape(T, PEER_HEADS, PEER_TOPK * PEER_TOPK)
    best_s, best_pos = lax.top_k(cand_s, PEER_TOPK)
    idx = jnp.take_along_axis(cand_i, best_pos, axis=-1)
    g = jax.nn.softmax(best_s, axis=-1).astype(h.dtype)
    nb = T // PEER_BLOCK
    u_tab, v_tab = p['peer_u'], p['peer_v']

    def block(args):
        xb, ib, gb = args
        act = jax.nn.gelu(jnp.einsum('td,thkd->thk', xb, u_tab[ib]), approximate=False)
        return jnp.einsum('thk,thkd->td', gb * act, v_tab[ib])

    out = lax.map(block, (hf.reshape(nb, PEER_BLOCK, D),
                          idx.reshape(nb, PEER_BLOCK, PEER_HEADS, PEER_TOPK),
                          g.reshape(nb, PEER_BLOCK, PEER_HEADS, PEER_TOPK)))
    return out.reshape(B, L, D)


def setup_inputs(seed: int = 0) -> dict:
    key = jax.random.key(seed)
    ks = jax.random.split(key, 32)
    f32 = jnp.float32

    def nrm(k, shape, scale):
        return jax.random.normal(k, shape, f32) * scale

    Ld = DEPTH
    return {
        'x': nrm(ks[0], (BATCH, SEQ, D_MODEL), 1.0),
        'c': nrm(ks[1], (BATCH, D_MODEL), 1.0),
        'ctx': nrm(ks[2], (BATCH, CTX_LEN, D_MODEL), 1.0),
        'c_ctx': nrm(ks[3], (D_MODEL,), 1.0),
        'norm1_g': 1.0 + nrm(ks[4], (Ld, D_MODEL), 0.02),
        'norm2_g': 1.0 + nrm(ks[5], (Ld, D_MODEL), 0.02),
        'w_ada': nrm(ks[6], (Ld, D_MODEL, N_MOD * D_MODEL), D_MODEL ** -0.5),
        'b_ada': nrm(ks[7], (Ld, N_MOD * D_MODEL), 0.02),
        'w_in': nrm(ks[8], (Ld, D_MODEL, N_PROJ), D_MODEL ** -0.5),
        'b_in': nrm(ks[9], (Ld, N_PROJ), 0.02),
        'hy_conv_w': nrm(ks[10], (Ld, 3, 3 * D_HY), 3 ** -0.5),
        'hy_conv_b': nrm(ks[11], (Ld, 3 * D_HY), 0.02),
        'hy_f1_w': nrm(ks[12], (Ld, HY_EMB, HY_FFN), HY_EMB ** -0.5),
        'hy_f1_b': nrm(ks[13], (Ld, HY_FFN), 0.02),
        'hy_f2_w': nrm(ks[14], (Ld, HY_FFN, HY_FFN), HY_FFN ** -0.5),
        'hy_f2_b': nrm(ks[15], (Ld, HY_FFN), 0.02),
        'hy_f3_w': nrm(ks[16], (Ld, HY_FFN, HY_FFN), HY_FFN ** -0.5),
        'hy_f3_b': nrm(ks[17], (Ld, HY_FFN), 0.02),
        'hy_f4_w': nrm(ks[18], (Ld, HY_FFN, HY_ORDER * 2 * D_HY), HY_FFN ** -0.5),
        'hy_sin_freq': 1.0 + nrm(ks[19], (Ld, HY_FFN), 0.02),
        'hy_skip': nrm(ks[20], (Ld, HY_ORDER, D_HY), 0.5),
        'q_norm_g': 1.0 + nrm(ks[21], (Ld, HEAD_DIM), 0.02),
        'k_norm_g': 1.0 + nrm(ks[22], (Ld, HEAD_DIM), 0.02),
        'na_rpb': nrm(ks[23], (Ld, NA_HEADS, 2 * WIN_ROWS - 1, 2 * WIN_COLS - 1), 0.02),
        'w_hy_out': nrm(ks[24], (Ld, D_HY, D_MODEL), D_HY ** -0.5),
        'w_na_out': nrm(ks[25], (Ld, D_NA, D_MODEL), D_NA ** -0.5),
        'w_out': nrm(ks[26], (Ld, D_MODEL, D_MODEL), D_MODEL ** -0.5),
        'peer_w_q': nrm(ks[27], (Ld, D_MODEL, PEER_HEADS * PEER_D_KEY), D_MODEL ** -0.5),
        'peer_keys': nrm(ks[28], (Ld, PEER_HEADS, 2, PEER_N_KEYS, PEER_D_HALF), PEER_D_HALF ** -0.5),
        'peer_u': nrm(ks[29], (Ld, PEER_N_EXPERTS, D_MODEL), D_MODEL ** -0.5),
        'peer_v': nrm(ks[30], (Ld, PEER_N_EXPERTS, D_MODEL), 0.5),
    }


def reference(x, c, ctx, c_ctx, norm1_g, norm2_g, w_ada, b_ada, w_in, b_in, hy_conv_w, hy_conv_b,
              hy_f1_w, hy_f1_b, hy_f2_w, hy_f2_b, hy_f3_w, hy_f3_b, hy_f4_w, hy_sin_freq, hy_skip,
              q_norm_g, k_norm_g, na_rpb, w_hy_out, w_na_out, w_out, peer_w_q, peer_keys, peer_u, peer_v):
    for layer in range(DEPTH):
        p = {
            'norm1_g': norm1_g[layer], 'norm2_g': norm2_g[layer],
            'w_ada': w_ada[layer], 'b_ada': b_ada[layer],
            'w_in': w_in[layer], 'b_in': b_in[layer],
            'hy_conv_w': hy_conv_w[layer], 'hy_conv_b': hy_conv_b[layer],
            'hy_f1_w': hy_f1_w[layer], 'hy_f1_b': hy_f1_b[layer],
            'hy_f2_w': hy_f2_w[layer], 'hy_f2_b': hy_f2_b[layer],
            'hy_f3_w': hy_f3_w[layer], 'hy_f3_b': hy_f3_b[layer],
            'hy_f4_w': hy_f4_w[layer], 'hy_sin_freq': hy_sin_freq[layer], 'hy_skip': hy_skip[layer],
            'q_norm_g': q_norm_g[layer], 'k_norm_g': k_norm_g[layer], 'na_rpb': na_rpb[layer],
            'w_hy_out': w_hy_out[layer], 'w_na_out': w_na_out[layer], 'w_out': w_out[layer],
            'peer_w_q': peer_w_q[layer], 'peer_keys': peer_keys[layer],
            'peer_u': peer_u[layer], 'peer_v': peer_v[layer],
        }
        m_lat = adaln(c, p)
        m_ctx = adaln(c_ctx, p)
        sh1, sc1, g1, sh2, sc2, g2 = [m_lat[:, i:i + 1] for i in range(N_MOD)]
        ch1, cs1, cg1, ch2, cs2, cg2 = [m_ctx[i:i + 1] for i in range(N_MOD)]

        h_ctx = modulate(ctx, p['norm1_g'], ch1, cs1)
        k_ctx, v_ctx = context_kv(h_ctx, p)

        def attend_latent(q, k, v, k_ctx=k_ctx, v_ctx=v_ctx, rpb=p['na_rpb']):
            return neighbourhood_attention(axial_rope(q), axial_rope(k), v, k_ctx, v_ctx, rpb)

        h = modulate(x, p['norm1_g'], sh1, sc1)
        x_next = x + g1 * token_mixer(h, p, attend_latent)
        x_next = x_next + g2 * peer(modulate(x_next, p['norm2_g'], sh2, sc2), p)

        if layer + 1 < DEPTH:
            ctx_next = ctx + cg1 * token_mixer(h_ctx, p, context_attention)
            ctx = ctx_next + cg2 * peer(modulate(ctx_next, p['norm2_g'], ch2, cs2), p)
        x = x_next
    return x
```

```python
import math
from contextlib import ExitStack

import numpy as np
import concourse.bass as bass
import concourse.mybir as mybir
from concourse.bass_utils import run_bass_kernel_spmd

F32 = mybir.dt.float32
BF16 = mybir.dt.bfloat16
U32 = mybir.dt.uint32
I32 = mybir.dt.int32
AF = mybir.ActivationFunctionType
ALU = mybir.AluOpType
AX = mybir.AxisListType

D = 1024
L = 8192
NPROJ = 5120
EPS = 1e-6
N_CORES = 8


class Res:
    __slots__ = ("name", "w", "r", "subs")

    def __init__(self, name="", subs=None):
        self.name = name
        self.w = None
        self.r = {}
        self.subs = subs


def _expand(lst):
    out = []
    for x in lst:
        if x.subs is not None:
            out.extend(x.subs)
        else:
            out.append(x)
    return out


class T:
    def __init__(self, t, name):
        self.t = t
        self.res = Res(name)

    def __getitem__(self, idx):
        return self.t[idx]


class Prog:
    EPOCH = 12000

    def __init__(self, nc, es):
        self.nc = nc
        self.es = es
        self.S = []
        self.engs = {"pe": nc.tensor, "act": nc.scalar, "dve": nc.vector, "pool": nc.gpsimd, "sp": nc.sync}
        self.cur = {}
        self.cnt = {}
        self.last = {}
        self.pe_sids = set()
        for e in ("pe", "act", "dve", "pool"):
            self.cur[e] = self._sem(e + "0")
            self.cnt[e] = 0
            self.last[e] = None
        self.pe_sids.add(self.cur["pe"])
        self.waited = {e: {} for e in self.engs}
        self.dq = {}
        self.dqi = {}
        for q, n in (("sp", 16), ("pool", 12), ("act", 6)):
            self.dq[q] = [[self._sem("d%s%d" % (q, i)), 0] for i in range(n)]
            self.dqi[q] = 0
        self.ninst = 0
        self._ps = None
        self._psi = 0
        self._psg = {}

    def _sem(self, name):
        h = self.es.enter_context(self.nc.semaphore(name))
        self.S.append(h)
        return len(self.S) - 1

    def wait(self, e, ev):
        sid, val = ev
        if self.waited[e].get(sid, 0) >= val:
            return
        self.engs[e].wait_ge(self.S[sid], val)
        self.waited[e][sid] = val

    @staticmethod
    def _deps(r, w):
        r, w = _expand(r), _expand(w)
        deps = {}
        for x in r:
            if x.w is not None and deps.get(x.w[0], 0) < x.w[1]:
                deps[x.w[0]] = x.w[1]
        for x in w:
            if x.w is not None and deps.get(x.w[0], 0) < x.w[1]:
                deps[x.w[0]] = x.w[1]
            for sid, val in x.r.items():
                if deps.get(sid, 0) < val:
                    deps[sid] = val
        return deps

    @staticmethod
    def _mark(ev, r, w):
        r, w = _expand(r), _expand(w)
        sid, val = ev
        for x in r:
            if x.r.get(sid, 0) < val:
                x.r[sid] = val
        for x in w:
            x.w = ev
            x.r = {}

    def op(self, e, fn, r=(), w=()):
        for sid, val in self._deps(r, w).items():
            if e == "pe" and sid in self.pe_sids:
                continue
            self.wait(e, (sid, val))
        inst = fn()
        if self.cnt[e] >= self.EPOCH:
            self.cur[e] = self._sem("%s%d" % (e, len(self.S)))
            self.cnt[e] = 0
            if e == "pe":
                self.pe_sids.add(self.cur[e])
        self.cnt[e] += 1
        inst.then_inc(self.S[self.cur[e]], 1)
        ev = (self.cur[e], self.cnt[e])
        self.last[e] = ev
        self._mark(ev, r, w)
        self.ninst += 1
        return inst

    def dma(self, q, out, in_, r=(), w=(), **kw):
        slot = self.dq[q][self.dqi[q]]
        self.dqi[q] = (self.dqi[q] + 1) % len(self.dq[q])
        sid, tot = slot
        if tot:
            self.wait(q, (sid, tot))
        for dep in self._deps(r, w).items():
            self.wait(q, dep)
        inst = self.engs[q].dma_start(out=out, in_=in_, **kw)
        inst.then_inc(self.S[sid], 16)
        slot[1] = tot + 16
        self._mark((sid, tot + 16), r, w)
        self.ninst += 1
        return inst

    def barrier(self):
        evs = [ev for ev in self.last.values() if ev is not None]
        for q in self.dq:
            for sid, tot in self.dq[q]:
                if tot:
                    evs.append((sid, tot))
        for e in self.engs:
            for ev in evs:
                self.wait(e, ev)

    def sb(self, es, name, shape, dtype):
        return T(es.enter_context(self.nc.sbuf_tensor("s_" + name, list(shape), dtype)), name)

    def dram(self, name, shape, dtype, kind="Internal"):
        return T(self.nc.dram_tensor("d_" + name, list(shape), dtype, kind=kind), name)

    def init_psum(self, es):
        self._ps = [T(es.enter_context(self.nc.psum_tensor("psb%d" % i, [128, 512], F32)), "psb%d" % i) for i in range(8)]

    def psum(self, grp=None):
        if grp is None:
            p = self._ps[self._psi]
            self._psi = (self._psi + 1) % 8
            return p
        k = self._psg.get(grp, 0)
        self._psg[grp] = (k + 1) % 4
        return self._ps[k + (0 if grp == "acc" else 4)]


def _bf(ap_tensor):
    return ap_tensor.bitcast(BF16)


def build_program(debug=None):
    debug = debug or set()
    nc = bass.Bass("TRN2", target_bir_lowering=False)
    es = ExitStack()
    P = Prog(nc, es)
    P.init_psum(es)

    def din(name, shape, dt=F32):
        return T(nc.dram_tensor(name, list(shape), dt, kind="ExternalInput"), name)

    def dout(name, shape, dt=F32):
        return T(nc.dram_tensor(name, list(shape), dt, kind="ExternalOutput"), name)

    xroll = din("xroll", [L, D])
    cT = din("cT", [128, 8, 2])
    w_ada = din("w_ada", [D, 6 * D])
    b_adaT = din("b_adaT", [128, 48])
    b_ada_g = din("b_ada_g", [2, D])
    n1T = din("n1T", [128, 8])
    n2T = din("n2T", [128, 8])
    w_in = din("w_in", [D, NPROJ])
    b_inT = din("b_inT", [128, 40])
    b_in_qkv = din("b_in_qkv", [1, 1536])
    ident_in = din("ident", [128, 128])
    ctxb = din("ctxb", [256, D])
    t_FAu = din("t_FAu", [128, 2, 2, 2, 128], BF16)
    t_FAq = din("t_FAq", [64, 2, 2, 2, 128], BF16)
    t_FAf = din("t_FAf", [128, 2, 2, 2, 128], BF16)
    t_DFu = din("t_DFu", [128, 3, 128], BF16)
    t_DFq = din("t_DFq", [128, 3, 64], BF16)
    t_twb = din("t_twb", [128, 2, 2, 128], BF16)
    t_sel = din("t_sel", [128, 128])
    t_omf = din("t_omf", [128, 2, 4])
    t_featsT = din("t_featsT", [33, L])
    t_dec0 = din("t_dec0", [512, 512])
    t_decs = din("t_decs", [512, 16])
    hy_convw = din("hy_convw", [128, 12, 3])
    hy_convb = din("hy_convb", [128, 12])
    hy_skipc = din("hy_skipc", [128, 2, 4])
    hy_f1_w = din("hy_f1_w", [33, 64])
    hy_f2_w = din("hy_f2_w", [64, 64])
    hy_f3_w = din("hy_f3_w", [64, 64])
    hy_f4_w = din("hy_f4_w", [64, 2048])
    hy_fbc = din("hy_fbc", [64, 4])
    w_hy_out = din("w_hy_out", [512, D])
    w_na_out = din("w_na_out", [512, D])
    w_out = din("w_out", [D, D])
    peer_w_q = din("peer_w_q", [D, 2048])
    peer_keysT = din("peer_keysT", [128, 16, 128])
    peer_uTp = din("peer_uTp", [D, 128, 128])
    peer_v = din("peer_v", [16384, D])
    t_io128 = din("t_io128", [128, 128])
    biasx = din("biasx", [8, 128, 8, 512])
    negmask = din("negmask", [4, 128, 8, 512])
    q_norm_g = din("q_norm_g", [1, 64])
    k_norm_g = din("k_norm_g", [1, 64])
    rope = din("rope", [20 * 128, 64])
    out = dout("out", [2048, D])

    dbg = {}

    def dbg_out(name, shape, dt=F32):
        dbg[name] = dout("dbg_" + name, shape, dt)
        return dbg[name]

    ubd = nc.dram_tensor("d_ubd", [32, 128, 4, 8, 128], BF16, kind="Internal")
    vbd = nc.dram_tensor("d_vbd", [32, 128, 4, D], BF16, kind="Internal")
    ubd_res = Res("ubd")
    vbd_res = Res("vbd")
    zhyT = [P.dram("zhyT%d" % m, [128, L], BF16) for m in range(12)]

    ident = P.sb(es, "ident", [128, 128], BF16)
    identf = P.sb(es, "identf", [128, 128], F32)
    modT = P.sb(es, "modT", [128, 48, 2], F32)
    mods = P.sb(es, "mods", [128, 8, 8], F32)
    MI_G1, MI_S1, MI_GC, MI_SC, MI_G2, MI_S2 = 0, 1, 2, 3, 4, 5
    grow = P.sb(es, "grow", [128, 2, D], F32)

    P.dma("sp", identf[:], ident_in[:, :], w=[identf.res])
    P.op("dve", lambda: nc.vector.tensor_copy(out=ident[:], in_=identf[:]), r=[identf.res], w=[ident.res])

    with ExitStack() as pa:
        cTs = P.sb(pa, "cTs", [128, 8, 2], F32)
        scT = P.sb(pa, "scT", [128, 8, 2], F32)
        screp = P.sb(pa, "screp", [128, 8, 128], F32)
        badaT = P.sb(pa, "badaT", [128, 48], F32)
        bgrow = P.sb(pa, "bgrow", [128, 2, D], F32)
        n1s = P.sb(pa, "n1s", [128, 8], F32)
        n2s = P.sb(pa, "n2s", [128, 8], F32)
        wa = [P.sb(pa, "wa%d" % i, [128, 8, D], F32) for i in range(2)]
        P.dma("sp", cTs[:], cT[:, :, :], w=[cTs.res])
        P.dma("sp", badaT[:], b_adaT[:, :], w=[badaT.res])
        P.dma("sp", n1s[:], n1T[:, :], w=[n1s.res])
        P.dma("sp", n2s[:], n2T[:, :], w=[n2s.res])
        for gi in range(2):
            P.dma("sp", bgrow[:, gi, :], b_ada_g[gi:gi + 1, :].partition_broadcast(128), w=[bgrow.res])
        P.op("act", lambda: nc.scalar.activation(out=scT[:], in_=cTs[:], func=AF.Silu), r=[cTs.res], w=[scT.res])
        P.op("dve", lambda: nc.vector.tensor_copy(out=screp[:], in_=scT[:, :, 0:1].to_broadcast([128, 8, 128])),
             r=[scT.res], w=[screp.res])
        psm = P.psum()
        for i in range(6):
            wt = wa[i % 2]
            P.dma("sp", wt[:], w_ada[:, i * D:(i + 1) * D].rearrange("(kc p) n -> p kc n", p=128), w=[wt.res])
            for dc in range(8):
                j = i * 8 + dc
                for kc in range(8):
                    P.op("pe", lambda: nc.tensor.matmul(psm[:, 2 * j:2 * j + 2], lhsT=wt[:, kc, dc * 128:(dc + 1) * 128],
                                                        rhs=scT[:, kc, :], start=(kc == 0), stop=(kc == 7)),
                         r=[wt.res, scT.res], w=[psm.res])
            if i in (2, 5):
                gi = 0 if i == 2 else 1
                for half in range(2):
                    pr = P.psum()
                    for kc in range(8):
                        P.op("pe", lambda: nc.tensor.matmul(pr[:, :], lhsT=screp[:, kc, :],
                                                            rhs=wt[:, kc, half * 512:(half + 1) * 512],
                                                            start=(kc == 0), stop=(kc == 7)),
                             r=[wt.res, screp.res], w=[pr.res])
                    P.op("dve", lambda: nc.vector.tensor_tensor(out=grow[:, gi, half * 512:(half + 1) * 512], in0=pr[:, :],
                                                                in1=bgrow[:, gi, half * 512:(half + 1) * 512], op=ALU.add),
                         r=[pr.res, bgrow.res], w=[grow.res])
        for col in range(2):
            P.op("dve", lambda: nc.vector.tensor_tensor(out=modT[:, :, col], in0=psm[:, 0:96].rearrange("p (j c) -> p j c", c=2)[:, :, col],
                                                        in1=badaT[:], op=ALU.add),
                 r=[psm.res, badaT.res], w=[modT.res])

        def geff(dst, ng, sc_idx, col):
            P.op("dve", lambda: nc.vector.tensor_scalar(out=mods[:, dst, :], in0=modT[:, sc_idx * 8:(sc_idx + 1) * 8, col],
                                                        scalar1=1.0, scalar2=None, op0=ALU.add),
                 r=[modT.res], w=[mods.res])
            P.op("dve", lambda: nc.vector.tensor_tensor(out=mods[:, dst, :], in0=mods[:, dst, :], in1=ng[:], op=ALU.mult),
                 r=[mods.res, ng.res], w=[mods.res])

        def cpy(dst, idx, col):
            P.op("dve", lambda: nc.vector.tensor_copy(out=mods[:, dst, :], in_=modT[:, idx * 8:(idx + 1) * 8, col]),
                 r=[modT.res], w=[mods.res])

        geff(MI_G1, n1s, 1, 0)
        cpy(MI_S1, 0, 0)
        geff(MI_GC, n1s, 1, 1)
        cpy(MI_SC, 0, 1)
        geff(MI_G2, n2s, 4, 0)
        cpy(MI_S2, 3, 0)
        P.barrier()

    if "modT" in debug:
        d1 = dbg_out("modT", [128, 96])
        d2 = dbg_out("grow", [128, 2 * D])
        P.dma("sp", d1[:, :], modT[:].rearrange("p j c -> p (j c)"), r=[modT.res], w=[d1.res])
        P.dma("sp", d2[:, :], grow[:].rearrange("p g n -> p (g n)"), r=[grow.res], w=[d2.res])

    if "stopA" in debug:
        P.barrier()
        return nc, es, dbg

    NA_T = 22
    attTd = P.dram("attTd", [128, 4, 2048], BF16)
    pbc = ExitStack()
    qT = P.sb(pbc, "qT", [128, 4, 2048], BF16)
    kT = P.sb(pbc, "kT", [128, 4, NA_T * 128], BF16)
    vz = P.sb(pbc, "vz", [128, NA_T, 512], BF16)
    gsT = [P.dram("gsT%d" % m, [128, 2048], BF16) for m in range(16)]

    with ExitStack() as pb:
        winb = P.sb(pb, "winb", [128, 8, NPROJ], BF16)
        binT = P.sb(pb, "binT", [128, 40], F32)
        bqkv = P.sb(pb, "bqkv", [128, 1536], F32)
        qg = P.sb(pb, "qg", [128, 64], F32)
        kg = P.sb(pb, "kg", [128, 64], F32)
        P.dma("sp", binT[:], b_inT[:, :], w=[binT.res])
        P.dma("sp", bqkv[:], b_in_qkv[0:1, :].partition_broadcast(128), w=[bqkv.res])
        P.dma("sp", qg[:], q_norm_g[0:1, :].partition_broadcast(128), w=[qg.res])
        P.dma("sp", kg[:], k_norm_g[0:1, :].partition_broadcast(128), w=[kg.res])
        xt = [P.sb(pb, "xt%d" % i, [128, D], F32) for i in range(2)]
        for pc in range(40):
            ws = xt[pc % 2]
            P.dma("sp", ws[:].rearrange("p (kc n) -> p kc n", kc=8), w_in[:, pc * 128:(pc + 1) * 128].rearrange("(kc p) n -> p kc n", p=128), w=[ws.res])
            eng = "dve" if pc % 2 == 0 else "pool"
            P.op(eng, lambda: P.engs[eng].tensor_copy(out=winb[:, :, pc * 128:(pc + 1) * 128], in_=ws[:].rearrange("p (kc n) -> p kc n", kc=8)),
                 r=[ws.res], w=[winb.res])

        junk = P.sb(pb, "junk", [128, D], BF16)
        xn = [P.sb(pb, "xn%d" % i, [128, D], BF16) for i in range(2)]
        st4 = [P.sb(pb, "st4_%d" % i, [128, 4], F32) for i in range(2)]
        hTb = [P.sb(pb, "hT%d" % i, [128, 8, 512], BF16) for i in range(2)]
        stg = [P.sb(pb, "stg%d" % i, [128, 512], BF16) for i in range(4)]
        qkv = [P.sb(pb, "qkv%d" % i, [128, 1536], F32) for i in range(1)]
        sq = P.sb(pb, "sq", [128, 512], F32)
        st8 = P.sb(pb, "st8", [128, 4, 8], F32)
        qn = P.sb(pb, "qn", [128, 512], F32)
        rt = [P.sb(pb, "rt%d" % i, [128, 4, 256], F32) for i in range(1)]
        qr = [P.sb(pb, "qr%d" % i, [128, 512], BF16) for i in range(2)]
        ropet = [P.sb(pb, "ropet%d" % i, [128, 64], F32) for i in range(2)]
        cnt = {"stg": 0, "ev": 0, "tile": 0, "qk": 0}

        def norm_tile(src_ap, gi, si, hT, tt):
            k = cnt["tile"]
            cnt["tile"] += 1
            x_ = xt[k % 2]
            xn_ = xn[k % 2]
            s_ = st4[k % 2]
            P.dma("sp", x_[:], src_ap, w=[x_.res])
            P.op("act", lambda: nc.scalar.activation(out=junk[:], in_=x_[:], func=AF.Square, accum_out=s_[:, 0:1]),
                 r=[x_.res], w=[junk.res, s_.res])
            P.op("dve", lambda: nc.vector.tensor_scalar(out=s_[:, 1:2], in0=s_[:, 0:1], scalar1=1.0 / D, scalar2=EPS,
                                                        op0=ALU.mult, op1=ALU.add), r=[s_.res], w=[s_.res])
            P.op("act", lambda: nc.scalar.activation(out=s_[:, 2:3], in_=s_[:, 1:2], func=AF.Sqrt), r=[s_.res], w=[s_.res])
            P.op("dve", lambda: nc.vector.reciprocal(out=s_[:, 3:4], in_=s_[:, 2:3]), r=[s_.res], w=[s_.res])
            P.op("dve", lambda: nc.vector.tensor_scalar(out=xn_[:], in0=x_[:], scalar1=s_[:, 3:4], scalar2=None, op0=ALU.mult),
                 r=[x_.res, s_.res], w=[xn_.res])
            pb_ = P.psum()
            pbf = pb_.t.bitcast(BF16)
            for kc in range(8):
                P.op("pe", lambda: nc.tensor.transpose(out=pbf[:, kc * 128:(kc + 1) * 128], in_=xn_[:, kc * 128:(kc + 1) * 128],
                                                       identity=ident[:]), r=[xn_.res, ident.res], w=[pb_.res])
            for kc in range(8):
                P.op("act", lambda: nc.scalar.activation(out=hT[:, kc, tt * 128:(tt + 1) * 128], in_=pbf[:, kc * 128:(kc + 1) * 128],
                                                         func=AF.Identity, scale=mods[:, gi, kc:kc + 1], bias=mods[:, si, kc:kc + 1]),
                     r=[pb_.res, mods.res], w=[hT.res])

        def fm_chunk(hT, ncols, m, func, dst_ap, dst_res):
            ps = P.psum()
            for kc in range(8):
                P.op("pe", lambda: nc.tensor.matmul(ps[:, :ncols], lhsT=winb[:, kc, m * 128:(m + 1) * 128], rhs=hT[:, kc, :ncols],
                                                    start=(kc == 0), stop=(kc == 7)), r=[winb.res, hT.res], w=[ps.res])
            sg = stg[cnt["stg"] % 4]
            cnt["stg"] += 1
            if func is None and cnt["ev"] % 2 == 0:
                P.op("dve", lambda: nc.vector.tensor_scalar(out=sg[:, :ncols], in0=ps[:, :ncols], scalar1=binT[:, m:m + 1], scalar2=None,
                                                            op0=ALU.add), r=[ps.res, binT.res], w=[sg.res])
            else:
                P.op("act", lambda: nc.scalar.activation(out=sg[:, :ncols], in_=ps[:, :ncols], func=(func or AF.Identity),
                                                         bias=binT[:, m:m + 1]), r=[ps.res, binT.res], w=[sg.res])
            cnt["ev"] += 1
            P.dma("pool", dst_ap, sg[:, :ncols], r=[sg.res], w=[dst_res])

        def headnorm_rope(src, qkvc, gain, rope_ap, dst):
            P.op("act", lambda: nc.scalar.activation(out=sq[:], in_=src, func=AF.Square), r=[qkvc.res], w=[sq.res])
            P.op("dve", lambda: nc.vector.tensor_reduce(out=st8[:, 0, :], in_=sq[:].rearrange("p (h d) -> p h d", d=64),
                                                        axis=AX.X, op=ALU.add), r=[sq.res], w=[st8.res])
            P.op("dve", lambda: nc.vector.tensor_scalar(out=st8[:, 1, :], in0=st8[:, 0, :], scalar1=1.0 / 64, scalar2=EPS,
                                                        op0=ALU.mult, op1=ALU.add), r=[st8.res], w=[st8.res])
            P.op("act", lambda: nc.scalar.activation(out=st8[:, 2, :], in_=st8[:, 1, :], func=AF.Sqrt), r=[st8.res], w=[st8.res])
            P.op("dve", lambda: nc.vector.reciprocal(out=st8[:, 3, :], in_=st8[:, 2, :]), r=[st8.res], w=[st8.res])
            qn3 = qn[:].rearrange("p (h d) -> p h d", d=64)
            P.op("dve", lambda: nc.vector.tensor_tensor(out=qn3, in0=src.rearrange("p (h d) -> p h d", d=64),
                                                        in1=st8[:, 3, :].unsqueeze(2).to_broadcast([128, 8, 64]), op=ALU.mult),
                 r=[qkvc.res, st8.res], w=[qn.res])
            if rope_ap is None:
                P.op("dve", lambda: nc.vector.tensor_tensor(out=dst[:].rearrange("p (h d) -> p h d", d=64), in0=qn3,
                                                            in1=gain[:].unsqueeze(1).to_broadcast([128, 8, 64]), op=ALU.mult),
                     r=[qn.res, gain.res], w=[dst.res])
                return
            P.op("dve", lambda: nc.vector.tensor_tensor(out=qn3, in0=qn3, in1=gain[:].unsqueeze(1).to_broadcast([128, 8, 64]),
                                                        op=ALU.mult), r=[qn.res, gain.res], w=[qn.res])
            q5 = qn[:].rearrange("p (h a b f) -> p h a b f", h=8, a=2, b=2)
            d5 = dst[:].rearrange("p (h a b f) -> p h a b f", h=8, a=2, b=2)
            A, B_ = q5[:, :, :, 0, :], q5[:, :, :, 1, :]
            C = rope_ap[:, 0:32].rearrange("p (a f) -> p a f", a=2).unsqueeze(1).to_broadcast([128, 8, 2, 16])
            S_ = rope_ap[:, 32:64].rearrange("p (a f) -> p a f", a=2).unsqueeze(1).to_broadcast([128, 8, 2, 16])
            r_ = rt[0]
            tv = [r_[:, i, :].rearrange("p (h a f) -> p h a f", h=8, a=2) for i in range(4)]
            for o_, i0, i1 in ((tv[0], A, C), (tv[1], B_, S_), (tv[2], A, S_), (tv[3], B_, C)):
                P.op("dve", lambda: nc.vector.tensor_tensor(out=o_, in0=i0, in1=i1, op=ALU.mult),
                     r=[qn.res, rope_ap.res], w=[r_.res])
            P.op("dve", lambda: nc.vector.tensor_tensor(out=d5[:, :, :, 0, :], in0=tv[0], in1=tv[1], op=ALU.subtract),
                 r=[r_.res], w=[dst.res])
            P.op("dve", lambda: nc.vector.tensor_tensor(out=d5[:, :, :, 1, :], in0=tv[2], in1=tv[3], op=ALU.add),
                 r=[r_.res], w=[dst.res])

        def to_T(src_bf, dstT, col0):
            ps = P.psum()
            pbf = ps.t.bitcast(BF16)
            for a in range(4):
                P.op("pe", lambda: nc.tensor.transpose(out=pbf[:, a * 128:(a + 1) * 128], in_=src_bf[:, a * 128:(a + 1) * 128],
                                                       identity=ident[:]), r=[src_bf.res, ident.res], w=[ps.res])
            P.op("act", lambda: nc.scalar.copy(out=dstT[:, :, col0:col0 + 128], in_=pbf[:, 0:512].rearrange("p (a t) -> p a t", a=4)),
                 r=[ps.res], w=[dstT.res])

        def tm_qkv(hT, tt, nb, parts, is_ctx):
            qkvc = qkv[0]
            ropec = ropet[cnt["qk"] % 2]
            cnt["qk"] += 1
            for part in parts:
                ps = P.psum()
                for kc in range(8):
                    P.op("pe", lambda: nc.tensor.matmul(ps[:, :], lhsT=hT[:, kc, tt * 128:(tt + 1) * 128],
                                                        rhs=winb[:, kc, 1536 + part * 512:1536 + (part + 1) * 512],
                                                        start=(kc == 0), stop=(kc == 7)), r=[winb.res, hT.res], w=[ps.res])
                P.op("dve", lambda: nc.vector.tensor_tensor(out=qkvc[:, part * 512:(part + 1) * 512], in0=ps[:, :],
                                                            in1=bqkv[:, part * 512:(part + 1) * 512], op=ALU.add),
                     r=[ps.res, bqkv.res], w=[qkvc.res])
            if not is_ctx:
                P.dma("sp", ropec[:], rope[nb * 128:(nb + 1) * 128, :], w=[ropec.res])
            if 0 in parts:
                qd = qr[0]
                headnorm_rope(qkvc[:, 0:512], qkvc, qg, ropec, qd)
                to_T(qd, qT, (nb - 2) * 128)
            kd = qr[1]
            headnorm_rope(qkvc[:, 512:1024], qkvc, kg, (None if is_ctx else ropec), kd)
            to_T(kd, kT, nb * 128)
            P.op("pool", lambda: nc.gpsimd.tensor_copy(out=vz[:, nb, :], in_=qkvc[:, 1024:1536]), r=[qkvc.res], w=[vz.res])

        for blk in range(16):
            hT = hTb[blk % 2]
            for tt in range(4):
                i = blk * 4 + tt
                norm_tile(xroll[i * 128:(i + 1) * 128, :], MI_G1, MI_S1, hT, tt)
            for m in range(12):
                fm_chunk(hT, 512, m, None, zhyT[m][:, blk * 512:(blk + 1) * 512], zhyT[m].res)
            if blk < 4:
                for m in range(24, 40):
                    fm_chunk(hT, 512, m, AF.Sigmoid, gsT[m - 24][:, blk * 512:(blk + 1) * 512], gsT[m - 24].res)
            for tt in range(4):
                i = blk * 4 + tt
                if i <= 17:
                    tm_qkv(hT, tt, i + 2, (0, 1, 2) if i < 16 else (1, 2), False)
                elif i >= 62:
                    tm_qkv(hT, tt, i - 62, (1, 2), False)
        hT = hTb[0]
        for tt in range(2):
            norm_tile(ctxb[tt * 128:(tt + 1) * 128, :], MI_GC, MI_SC, hT, tt)
        for tt in range(2):
            tm_qkv(hT, tt, 20 + tt, (1, 2), True)
        P.barrier()

    if "phaseB" in debug:
        d1 = dbg_out("zhyT", [12 * 128, L], BF16)
        d2 = dbg_out("gsT", [16 * 128, 2048], BF16)
        d3 = dbg_out("qT", [128, 4 * 2048], BF16)
        d4 = dbg_out("kT", [128, 4 * NA_T * 128], BF16)
        d5 = dbg_out("vz", [128, NA_T * 512], BF16)
        for m in range(12):
            P.dma("sp", d1[m * 128:(m + 1) * 128, :], zhyT[m][:, :], r=[zhyT[m].res], w=[d1.res])
        for m in range(16):
            P.dma("sp", d2[m * 128:(m + 1) * 128, :], gsT[m][:, :], r=[gsT[m].res], w=[d2.res])
        P.dma("sp", d3[:, :], qT[:].rearrange("p a t -> p (a t)"), r=[qT.res], w=[d3.res])
        P.dma("sp", d4[:, :], kT[:].rearrange("p a t -> p (a t)"), r=[kT.res], w=[d4.res])
        P.dma("sp", d5[:, :], vz[:].rearrange("p t d -> p (t d)"), r=[vz.res], w=[d5.res])
    if "stopB" in debug:
        P.barrier()
        return nc, es, dbg

    with ExitStack() as pc_:
        attT = P.sb(pc_, "attT", [128, 4, 2048], BF16)
        onesb = P.sb(pc_, "onesb", [128, 128], BF16)
        P.op("dve", lambda: nc.vector.memset(onesb[:], 1.0), w=[onesb.res])
        bx = [P.sb(pc_, "bx%d" % i, [128, 8, 512], F32) for i in range(2)]
        nm = P.sb(pc_, "nm", [128, 8, 512], F32)
        bm = [P.sb(pc_, "bm%d" % i, [128, 8, 512], F32) for i in range(2)]
        tt_ = [P.sb(pc_, "ttc%d" % i, [128, 512], F32) for i in range(2)]
        pT = [P.sb(pc_, "pT%d" % i, [128, 512], BF16) for i in range(3)]
        rden = P.sb(pc_, "rden", [128, 512], F32)
        cvf = [P.sb(pc_, "cvf%d" % i, [128, 2048], F32) for i in range(2)]
        cvb = [P.sb(pc_, "cvb%d" % i, [128, 2048], BF16) for i in range(2)]
        cvn = {"k": 0}

        def conv_steps(n):
            for _ in range(n):
                k = cvn["k"]
                if k >= 128:
                    return
                cvn["k"] += 1
                f_, b_ = cvf[k % 2], cvb[k % 2]
                if k < 64:
                    for j2 in range(2):
                        P.dma("sp", f_[:, j2 * 1024:(j2 + 1) * 1024].rearrange("p (kc i) -> p kc i", kc=8),
                              peer_uTp[:, 2 * k + j2, :].rearrange("(kc p) i -> p kc i", p=128), w=[f_.res])
                else:
                    P.dma("sp", f_[:].rearrange("p (j d) -> p j d", j=2),
                          peer_v.t.ap().rearrange("(i1 i2) d -> i1 i2 d", i2=128)[:, 2 * (k - 64):2 * (k - 64) + 2, :], w=[f_.res])
                P.op("pool", lambda: nc.gpsimd.tensor_copy(out=b_[:], in_=f_[:]), r=[f_.res], w=[b_.res])
                if k < 64:
                    P.dma("pool", ubd[k // 2, :, 2 * (k % 2):2 * (k % 2) + 2, :, :].rearrange("p j kc i -> p (j kc i)"), b_[:], r=[b_.res], w=[ubd_res])
                else:
                    kk = k - 64
                    P.dma("pool", vbd[kk // 2, :, 2 * (kk % 2):2 * (kk % 2) + 2, :].rearrange("p j d -> p (j d)"), b_[:], r=[b_.res], w=[vbd_res])

        it = 0
        for j in range(4):
            P.dma("sp", nm[:], negmask[j, :, :, :], w=[nm.res])
            for h in range(8):
                a, hb = h // 2, (h % 2) * 64
                bx_ = bx[it % 2]
                bm_ = bm[it % 2]
                it += 1
                P.dma("sp", bx_[:], biasx[h, :, :, :], w=[bx_.res])
                P.op("dve", lambda: nc.vector.tensor_tensor(out=bm_[:], in0=bx_[:], in1=nm[:], op=ALU.add),
                     r=[bx_.res, nm.res], w=[bm_.res])
                conv_steps(4)
                psN = P.psum("acc")
                psD = P.psum("acc")
                def s_c(c):
                    tile_i = (4 * j + c) if c < 8 else (20 + c - 8)
                    psS = P.psum("tmp")
                    P.op("pe", lambda: nc.tensor.matmul(psS[:, :], lhsT=kT[hb:hb + 64, a, tile_i * 128:(tile_i + 1) * 128],
                                                        rhs=qT[hb:hb + 64, a, j * 512:(j + 1) * 512], start=True, stop=True),
                         r=[kT.res, qT.res], w=[psS.res])
                    p_ = pT[c % 3]
                    if c < 8:
                        t_ = tt_[c % 2]
                        P.op("dve", lambda: nc.vector.scalar_tensor_tensor(out=t_[:], in0=psS[:, :], scalar=0.125, in1=bm_[:, c, :],
                                                                           op0=ALU.mult, op1=ALU.add),
                             r=[psS.res, bm_.res], w=[t_.res])
                        P.op("act", lambda: nc.scalar.activation(out=p_[:], in_=t_[:], func=AF.Exp), r=[t_.res], w=[p_.res])
                    else:
                        P.op("act", lambda: nc.scalar.activation(out=p_[:], in_=psS[:, :], func=AF.Exp, scale=0.125),
                             r=[psS.res], w=[p_.res])
                    return p_, tile_i

                def pv_c(c, st_):
                    p_, tile_i = st_
                    P.op("pe", lambda: nc.tensor.matmul(psN[hb:hb + 64, :], lhsT=vz[:, tile_i, h * 64:(h + 1) * 64], rhs=p_[:],
                                                        start=(c == 0), stop=(c == 9)), r=[vz.res, p_.res], w=[psN.res])
                    P.op("pe", lambda: nc.tensor.matmul(psD[:, :], lhsT=onesb[:], rhs=p_[:], start=(c == 0), stop=(c == 9)),
                         r=[onesb.res, p_.res], w=[psD.res])

                prev = None
                for c in range(10):
                    cur = s_c(c)
                    if prev is not None:
                        pv_c(c - 1, prev)
                    prev = cur
                pv_c(9, prev)
                P.op("dve", lambda: nc.vector.reciprocal(out=rden[hb:hb + 64, :], in_=psD[hb:hb + 64, :]), r=[psD.res], w=[rden.res])
                P.op("dve", lambda: nc.vector.tensor_tensor(out=attT[hb:hb + 64, a, j * 512:(j + 1) * 512], in0=psN[hb:hb + 64, :],
                                                            in1=rden[hb:hb + 64, :], op=ALU.mult),
                     r=[psN.res, rden.res], w=[attT.res])
        P.dma("sp", attTd.t.ap(), attT[:], r=[attT.res], w=[attTd.res])
        P.barrier()

    pbc.close()
    if "phaseC" in debug:
        d1 = dbg_out("attT", [128, 4 * 2048], BF16)
        P.dma("sp", d1[:, :], attTd.t.ap().rearrange("p a t -> p (a t)"), r=[attTd.res], w=[d1.res])
    if "stopC" in debug:
        P.barrier()
        return nc, es, dbg

    CS = 64
    NFFT = 16384
    hyTd = [P.dram("hyTd%d" % g, [128, 2048], BF16) for g in range(4)]
    with ExitStack() as pd:
        FAu = P.sb(pd, "FAu", [128, 2, 2, 2, 128], BF16)
        FAq = P.sb(pd, "FAq", [64, 2, 2, 2, 128], BF16)
        FAf = P.sb(pd, "FAf", [128, 2, 2, 2, 128], BF16)
        DFu = P.sb(pd, "DFu", [128, 3, 128], BF16)
        DFq = P.sb(pd, "DFq", [128, 3, 64], BF16)
        twb = P.sb(pd, "twb", [128, 2, 2, 128], BF16)
        sel = P.sb(pd, "sel", [128, 128], F32)
        convw = P.sb(pd, "convw", [128, 12, 3], F32)
        convb = P.sb(pd, "convb", [128, 12], F32)
        omf = P.sb(pd, "omf", [128, 2, 4], F32)
        wedge = P.sb(pd, "wedge", [128, 2, 12, 4], F32)
        skipc = P.sb(pd, "skipc", [128, 2, 4], F32)
        h3 = P.sb(pd, "h3", [64, L], BF16)
        W4b = P.sb(pd, "W4b", [64, 2048], BF16)
        for dst, src in ((FAu, t_FAu), (FAq, t_FAq), (FAf, t_FAf), (DFu, t_DFu), (DFq, t_DFq), (twb, t_twb), (sel, t_sel),
                         (convw, hy_convw), (convb, hy_convb), (omf, t_omf), (skipc, hy_skipc)):
            P.dma("sp", dst.t.ap(), src.t.ap(), w=[dst.res])
        for e_ in range(2):
            for m in range(12):
                P.op("dve", lambda: nc.vector.tensor_scalar(out=wedge[:, e_, m, :], in0=omf[:, e_, :], scalar1=convw[:, m, 2 * e_:2 * e_ + 1],
                                                            scalar2=None, op0=ALU.mult), r=[omf.res, convw.res], w=[wedge.res])

        with ExitStack() as pm:
            featsT = P.sb(pm, "featsT", [33, L], F32)
            hA = P.sb(pm, "hA", [64, L], F32)
            hB = P.sb(pm, "hB", [64, L], F32)
            W1 = P.sb(pm, "W1", [33, 64], F32)
            W2 = P.sb(pm, "W2", [64, 64], F32)
            W3 = P.sb(pm, "W3", [64, 64], F32)
            fbc = P.sb(pm, "fbc", [64, 4], F32)
            fb = P.sb(pm, "fb", [64, 3], F32)
            W4f = P.sb(pm, "W4f", [64, 2048], F32)
            ut = [P.sb(pm, "ut%d" % i, [64, 512], F32) for i in range(2)]
            kt = [P.sb(pm, "kt%d" % i, [64, 512], F32) for i in range(2)]
            P.dma("sp", featsT[:], t_featsT[:, :], w=[featsT.res])
            P.dma("sp", W1[:], hy_f1_w[:, :], w=[W1.res])
            P.dma("sp", W2[:], hy_f2_w[:, :], w=[W2.res])
            P.dma("sp", W3[:], hy_f3_w[:, :], w=[W3.res])
            P.dma("sp", fbc[:], hy_fbc[:, :], w=[fbc.res])
            P.dma("sp", W4f[:], hy_f4_w[:, :], w=[W4f.res])
            for o_ in range(2):
                P.op("dve", lambda: nc.vector.tensor_copy(
                    out=W4b[:, o_ * 1024:(o_ + 1) * 1024].rearrange("p (cc d c) -> p cc d c", cc=8, d=2),
                    in_=W4f[:, o_ * 1024:(o_ + 1) * 1024].rearrange("p (d cc c) -> p cc d c", d=2, cc=8)), r=[W4f.res], w=[W4b.res])
            P.op("dve", lambda: nc.vector.tensor_scalar(out=fb[:], in0=fbc[:, 0:3], scalar1=fbc[:, 3:4], scalar2=None, op0=ALU.mult),
                 r=[fbc.res], w=[fb.res])
            MAGIC = 12582912.0
            TWO_PI = 2.0 * math.pi

            def mlp_layer(src, K, Wt, li, dst):
                for blk in range(16):
                    cols = slice(blk * 512, (blk + 1) * 512)
                    ps = P.psum("tmp")
                    P.op("pe", lambda: nc.tensor.matmul(ps[:64, :], lhsT=Wt[:K, :], rhs=src[:K, cols], start=True, stop=True),
                         r=[Wt.res, src.res], w=[ps.res])
                    u = ut[blk % 2]
                    k_ = kt[blk % 2]
                    P.op("dve", lambda: nc.vector.tensor_scalar(out=u[:], in0=ps[:64, :], scalar1=fbc[:, 3:4], scalar2=fb[:, li:li + 1],
                                                                op0=ALU.mult, op1=ALU.add), r=[ps.res, fbc.res, fb.res], w=[u.res])
                    P.op("dve", lambda: nc.vector.tensor_scalar(out=k_[:], in0=u[:], scalar1=1.0 / TWO_PI, scalar2=MAGIC,
                                                                op0=ALU.mult, op1=ALU.add), r=[u.res], w=[k_.res])
                    P.op("dve", lambda: nc.vector.tensor_scalar(out=k_[:], in0=k_[:], scalar1=-MAGIC, scalar2=None, op0=ALU.add),
                         r=[k_.res], w=[k_.res])
                    P.op("dve", lambda: nc.vector.scalar_tensor_tensor(out=u[:], in0=k_[:], scalar=-TWO_PI, in1=u[:], op0=ALU.mult, op1=ALU.add),
                         r=[k_.res, u.res], w=[u.res])
                    P.op("dve", lambda: nc.vector.tensor_scalar(out=u[:], in0=u[:], scalar1=3.141592, scalar2=-3.141592,
                                                                op0=ALU.min, op1=ALU.max), r=[u.res], w=[u.res])
                    P.op("act", lambda: nc.scalar.activation(out=dst[:, cols], in_=u[:], func=AF.Sin), r=[u.res], w=[dst.res])

            mlp_layer(featsT, 33, W1, 0, hA)
            mlp_layer(hA, 64, W2, 1, hB)
            mlp_layer(hB, 64, W3, 2, h3)
            P.barrier()

        if "h3" in debug:
            d1 = dbg_out("h3", [64, L], BF16)
            P.dma("sp", d1[:, :], h3[:], r=[h3.res], w=[d1.res])

        vc = P.sb(pd, "vc", [128, L], BF16)
        x1c = P.sb(pd, "x1c", [128, L], BF16)
        y1 = P.sb(pd, "y1", [128, L], BF16)
        x2c = P.sb(pd, "x2c", [128, 2048], BF16)
        Xin = P.sb(pd, "Xin", [128, CS, 128], BF16)
        Abuf = P.sb(pd, "Abuf", [128, 2, CS, 64], BF16)
        Kf = P.sb(pd, "Kf", [128, 2, CS, 128], BF16)
        Ybuf = P.sb(pd, "Ybuf", [128, 2, CS, 128], BF16)
        hf = T(Ybuf[:, 0, :, :].rearrange("p c k -> p (c k)"), "hf")
        WK = [P.sb(pd, "WK%d" % i, [128, 2, 512], BF16) for i in range(4)]
        dec0 = P.sb(pd, "dec0", [128, 512], F32)
        decs = P.sb(pd, "decs", [128, 16], F32)
        acc = P.sb(pd, "acc", [128, 20], F32)
        rtot = P.sb(pd, "rtot", [128, 2], F32)
        junkh = P.sb(pd, "junkh", [128, 512], BF16)
        gt = [P.sb(pd, "gt%d" % i, [128, 1024], F32) for i in range(1)]
        ctmp = gt[0]
        cn = {"g": 0, "e": 0}
        def short_conv(src_rows, m, dst, nblk):
            P.dma("sp", hf[:, :], src_rows.t.ap(), r=[src_rows.res], w=[hf.res])
            W_ = 1024
            for blk in range(nblk):
                c0 = blk * W_
                P.op("act", lambda: nc.scalar.activation(out=ctmp[:, :], in_=hf[:, c0:c0 + W_], func=AF.Identity, scale=convw[:, m, 1:2],
                                                         bias=convb[:, m:m + 1]), r=[hf.res, convw.res, convb.res], w=[ctmp.res])
                P.op("dve", lambda: nc.vector.scalar_tensor_tensor(out=ctmp[:, 1:W_], in0=hf[:, c0:c0 + W_ - 1], scalar=convw[:, m, 0:1],
                                                                   in1=ctmp[:, 1:W_], op0=ALU.mult, op1=ALU.add),
                     r=[hf.res, convw.res, ctmp.res], w=[ctmp.res])
                P.op("dve", lambda: nc.vector.scalar_tensor_tensor(out=ctmp[:, 0:W_ - 1], in0=hf[:, c0 + 1:c0 + W_], scalar=convw[:, m, 2:3],
                                                                   in1=ctmp[:, 0:W_ - 1], op0=ALU.mult, op1=ALU.add),
                     r=[hf.res, convw.res, ctmp.res], w=[ctmp.res])
                lft = (c0 - 1) % L
                rgt = (c0 + W_) % L
                sl = wedge[:, 0, m, (c0 // 2048):(c0 // 2048) + 1] if c0 % 2048 == 0 else convw[:, m, 0:1]
                sr = wedge[:, 1, m, (c0 // 2048):(c0 // 2048) + 1] if (c0 + W_) % 2048 == 0 else convw[:, m, 2:3]
                P.op("dve", lambda: nc.vector.scalar_tensor_tensor(out=ctmp[:, 0:1], in0=hf[:, lft:lft + 1], scalar=sl,
                                                                   in1=ctmp[:, 0:1], op0=ALU.mult, op1=ALU.add),
                     r=[hf.res, wedge.res, convw.res, ctmp.res], w=[ctmp.res])
                P.op("dve", lambda: nc.vector.scalar_tensor_tensor(out=ctmp[:, W_ - 1:W_], in0=hf[:, rgt:rgt + 1], scalar=sr,
                                                                   in1=ctmp[:, W_ - 1:W_], op0=ALU.mult, op1=ALU.add),
                     r=[hf.res, wedge.res, convw.res, ctmp.res], w=[ctmp.res])
                P.op("act", lambda: nc.scalar.copy(out=dst[:, c0:c0 + W_], in_=ctmp[:, :]), r=[ctmp.res], w=[dst.res])

        def filter_gen(c0abs, o):
            lh = W4b[:, o * 1024 + (c0abs // 64) * 128:o * 1024 + (c0abs // 64) * 128 + 128]
            for blk in range(16):
                cols = slice(blk * 512, (blk + 1) * 512)
                ps = P.psum("tmp")
                P.op("pe", lambda: nc.tensor.matmul(ps[:, :], lhsT=lh, rhs=h3[:, cols], start=True, stop=True),
                     r=[W4b.res, h3.res], w=[ps.res])
                P.op("dve", lambda: nc.vector.scalar_tensor_tensor(out=hf[:, cols], in0=ps[:, :], scalar=decs[:, blk:blk + 1], in1=dec0[:],
                                                                   op0=ALU.mult, op1=ALU.mult), r=[ps.res, decs.res, dec0.res], w=[hf.res])
                P.op("act", lambda: nc.scalar.activation(out=junkh[:], in_=hf[:, cols], func=AF.Abs, accum_out=acc[:, blk:blk + 1]),
                     r=[hf.res], w=[junkh.res, acc.res])
            P.op("dve", lambda: nc.vector.tensor_reduce(out=acc[:, 16:17], in_=acc[:, 0:16], axis=AX.X, op=ALU.add), r=[acc.res], w=[acc.res])
            ps = P.psum("tmp")
            P.op("pe", lambda: nc.tensor.matmul(ps[:, 0:1], lhsT=sel[:], rhs=acc[:, 16:17], start=True, stop=True),
                 r=[sel.res, acc.res], w=[ps.res])
            P.op("dve", lambda: nc.vector.reciprocal(out=rtot[:, o:o + 1], in_=ps[:, 0:1]), r=[ps.res], w=[rtot.res])
            P.op("dve", lambda: nc.vector.memset(hf[64:128, 0:1], 0.0), w=[hf.res])

        def to_xin(S_, cs):
            Sv = S_[cs:cs + 64, :].rearrange("p (t l) -> p l t", l=128)
            for g16 in range(8):
                ps = P.psum("tmp")
                pbf = ps.t.bitcast(BF16)
                for s_ in range(16):
                    lo = g16 * 16 + s_
                    P.op("pe", lambda: nc.tensor.transpose(out=pbf[:64, s_ * 64:(s_ + 1) * 64], in_=Sv[:, lo, :], identity=ident[cs:cs + 64, cs:cs + 64]),
                         r=[S_.res, ident.res], w=[ps.res])
                eng = "act"
                o_ = Xin[:64, :, g16 * 16:(g16 + 1) * 16]
                i_ = pbf[:64, 0:1024].rearrange("p (l c) -> p c l", c=64)
                if eng == "act":
                    P.op("act", lambda: nc.scalar.copy(out=o_, in_=i_), r=[ps.res], w=[Xin.res])
                else:
                    P.op("dve", lambda: nc.vector.tensor_copy(out=o_, in_=i_), r=[ps.res], w=[Xin.res])

        AbR = [Res("Ab%d" % i) for i in range(CS // 8)]
        KfR = [Res("Kf%d" % i) for i in range(CS // 8)]
        YbR = [Res("Yb%d" % i) for i in range(CS // 8)]
        hf.res.subs = YbR

        def to_xin_filter():
            Sf = hf[0:64, :].rearrange("p (t l) -> p l t", l=128)
            Sb = hf[64:128, :].rearrange("p (t l) -> p l t", l=128)
            P.op("pool", lambda: nc.gpsimd.memset(Xin[96:128, :, 0:1], 0.0), w=[Xin.res])
            for g16 in range(8):
                ps = P.psum("tmp")
                pbf = ps.t.bitcast(BF16)
                for s_ in range(16):
                    lo = g16 * 16 + s_
                    P.op("pe", lambda: nc.tensor.transpose(out=pbf[0:64, s_ * 64:(s_ + 1) * 64], in_=Sf[:, lo, :], identity=ident[0:64, 0:64]),
                         r=[hf.res, ident.res], w=[ps.res])
                    if lo == 0:
                        P.op("pe", lambda: nc.tensor.transpose(out=pbf[64:127, 0:64], in_=Sb[:, 0, 1:64], identity=ident[64:128, 64:128]),
                             r=[hf.res, ident.res], w=[ps.res])
                    else:
                        P.op("pe", lambda: nc.tensor.transpose(out=pbf[64:128, s_ * 64:(s_ + 1) * 64], in_=Sb[:, 128 - lo, :], identity=ident[64:128, 64:128]),
                             r=[hf.res, ident.res], w=[ps.res])
                o_ = Xin[:, :, g16 * 16:(g16 + 1) * 16]
                i_ = pbf[:, 0:1024].rearrange("p (l c) -> p c l", c=64)
                if g16 == 0:
                    P.op("act", lambda: nc.scalar.copy(out=Xin[0:64, :, 0:16], in_=pbf[0:64, 0:1024].rearrange("p (l c) -> p c l", c=64)), r=[ps.res], w=[Xin.res])
                    P.op("dve", lambda: nc.vector.tensor_copy(out=Xin[64:128, :, 1:16], in_=pbf[64:128, 64:1024].rearrange("p (l c) -> p c l", c=64)), r=[ps.res], w=[Xin.res])
                    P.op("dve", lambda: nc.vector.tensor_copy(out=Xin[64:127, :, 0:1], in_=pbf[64:127, 0:64].rearrange("p (l c) -> p c l", c=64)), r=[ps.res], w=[Xin.res])
                else:
                    P.op("act", lambda: nc.scalar.copy(out=o_, in_=i_), r=[ps.res], w=[Xin.res])

        def stage_a(inp_fn, in_res_fn, K, FA, half):
            for c4 in range(CS // 4):
                psA = P.psum("tmp")
                for cc in range(4):
                    c = c4 * 4 + cc
                    lhs = inp_fn(c)
                    for vi, l_ in enumerate(lhs):
                        P.op("pe", lambda: nc.tensor.matmul(psA[:, cc * 128:(cc + 1) * 128], lhsT=l_, rhs=FA[:K, 0, vi, half, :],
                                                            start=(vi == 0), stop=(vi == len(lhs) - 1)),
                             r=[in_res_fn(c), FA.res], w=[psA.res])
                k = cn["g"]
                cn["g"] += 1
                a_ = WK[k % 2]
                m_ = WK[2 + k % 2]
                P.op("act", lambda: nc.scalar.copy(out=a_[:, 0, :], in_=psA[:, :]), r=[psA.res], w=[a_.res])
                en = "pool" if c4 in (2, 5, 7, 10, 13, 15) else "dve"
                eo = P.engs[en]
                for j in range(2):
                    P.op(en, lambda: eo.tensor_tensor(out=m_[:, j, :].rearrange("p (a k) -> p a k", k=64),
                                                      in0=a_[:, 0, :].rearrange("p (a k) -> p a k", k=64),
                                                      in1=twb[:, j, half, 0:64].unsqueeze(1).to_broadcast([128, 8, 64]), op=ALU.mult),
                         r=[a_.res, twb.res], w=[m_.res])
                m1 = m_[:, 0, :].rearrange("p (cc r k) -> p cc r k", cc=4, r=2)
                m2 = m_[:, 1, :].rearrange("p (cc r k) -> p cc r k", cc=4, r=2)
                P.op(en, lambda: eo.tensor_tensor(out=Abuf[:, 0, c4 * 4:c4 * 4 + 4, :], in0=m1[:, :, 0, :], in1=m2[:, :, 1, :], op=ALU.subtract),
                     r=[m_.res], w=[AbR[c4 // 2]])
                P.op(en, lambda: eo.tensor_tensor(out=Abuf[:, 1, c4 * 4:c4 * 4 + 4, :], in0=m1[:, :, 1, :], in1=m2[:, :, 0, :], op=ALU.add),
                     r=[m_.res], w=[AbR[c4 // 2]])

        def stage_b(DF, M, half, want_imag, evac):
            for cb in range(CS // 8):
                ar = Abuf[:, 0, cb * 8:(cb + 1) * 8, :]
                ai = Abuf[:, 1, cb * 8:(cb + 1) * 8, :]
                psR = P.psum("acc")
                P.op("pe", lambda: nc.tensor.matmul(psR[:M, :], lhsT=DF[:, 0, :M], rhs=ar, start=True, stop=False),
                     r=[DF.res, AbR[cb]], w=[psR.res])
                P.op("pe", lambda: nc.tensor.matmul(psR[:M, :], lhsT=DF[:, 2, :M], rhs=ai, start=False, stop=True),
                     r=[DF.res, AbR[cb]], w=[psR.res])
                psI = None
                if want_imag:
                    psI = P.psum("acc")
                    P.op("pe", lambda: nc.tensor.matmul(psI[:M, :], lhsT=DF[:, 1, :M], rhs=ar, start=True, stop=False),
                         r=[DF.res, AbR[cb]], w=[psI.res])
                    P.op("pe", lambda: nc.tensor.matmul(psI[:M, :], lhsT=DF[:, 0, :M], rhs=ai, start=False, stop=True),
                         r=[DF.res, AbR[cb]], w=[psI.res])
                evac(psR, psI, cb, half)

        def fft_pass(inp_fn, in_res_fn, K, FA, DF, M, want_imag, evac):
            for half in range(2):
                stage_a(inp_fn, in_res_fn, K, FA, half)
                stage_b(DF, M, half, want_imag, evac)

        INVN = 1.0 / NFFT

        def blk3(ps, M=128):
            return ps[:M, :].rearrange("p (c k) -> p c k", k=64)

        def evac_filt_first(psR, psI, cb, half):
            ks = slice(half * 64, (half + 1) * 64)
            cs_ = slice(cb * 8, (cb + 1) * 8)
            P.op("act", lambda: nc.scalar.mul(out=Kf[:, 0, cs_, ks], in_=blk3(psR), mul=INVN), r=[psR.res], w=[KfR[cb]])
            P.op("act", lambda: nc.scalar.mul(out=Kf[:, 1, cs_, ks], in_=blk3(psI), mul=-INVN), r=[psI.res], w=[KfR[cb]])

        def evac_filt_second(psR, psI, cb, half):
            ks = slice(half * 64, (half + 1) * 64)
            cs_ = slice(cb * 8, (cb + 1) * 8)
            for ri, ps_ in ((0, psR), (1, psI)):
                P.op("dve", lambda: nc.vector.scalar_tensor_tensor(out=Kf[:, ri, cs_, ks], in0=blk3(ps_), scalar=INVN, in1=Kf[:, ri, cs_, ks],
                                                                   op0=ALU.mult, op1=ALU.add), r=[ps_.res, KfR[cb]], w=[KfR[cb]])

        def evac_pointwise(psR, psI, cb, half):
            ks = slice(half * 64, (half + 1) * 64)
            cs_ = slice(cb * 8, (cb + 1) * 8)
            k = cn["e"]
            cn["e"] += 1
            x_ = WK[k % 2]
            ta, tb = WK[2], WK[3]
            P.op("act", lambda: nc.scalar.copy(out=x_[:, 0, :], in_=psR[:, :]), r=[psR.res], w=[x_.res])
            P.op("act", lambda: nc.scalar.copy(out=x_[:, 1, :], in_=psI[:, :]), r=[psI.res], w=[x_.res])
            v3 = lambda ap_: ap_.rearrange("p (c k) -> p c k", k=64)
            kr, nki = Kf[:, 0, cs_, ks], Kf[:, 1, cs_, ks]
            P.op("dve", lambda: nc.vector.tensor_tensor(out=v3(ta[:, 0, :]), in0=v3(x_[:, 0, :]), in1=kr, op=ALU.mult), r=[x_.res, KfR[cb]], w=[ta.res])
            P.op("dve", lambda: nc.vector.tensor_tensor(out=v3(ta[:, 1, :]), in0=v3(x_[:, 1, :]), in1=nki, op=ALU.mult), r=[x_.res, KfR[cb]], w=[ta.res])
            P.op("dve", lambda: nc.vector.tensor_tensor(out=Ybuf[:, 0, cs_, ks], in0=v3(ta[:, 0, :]), in1=v3(ta[:, 1, :]), op=ALU.add),
                 r=[ta.res], w=[YbR[cb]])
            P.op("dve", lambda: nc.vector.tensor_tensor(out=v3(tb[:, 0, :]), in0=v3(x_[:, 0, :]), in1=nki, op=ALU.mult), r=[x_.res, KfR[cb]], w=[tb.res])
            P.op("dve", lambda: nc.vector.tensor_tensor(out=v3(tb[:, 1, :]), in0=v3(x_[:, 1, :]), in1=kr, op=ALU.mult), r=[x_.res, KfR[cb]], w=[tb.res])
            P.op("dve", lambda: nc.vector.tensor_tensor(out=Ybuf[:, 1, cs_, ks], in0=v3(tb[:, 0, :]), in1=v3(tb[:, 1, :]), op=ALU.subtract),
                 r=[tb.res], w=[YbR[cb]])

        def make_evac_inv(M):
            def ev(psR, psI, cb, half):
                o_ = Xin[:M, cb * 8:(cb + 1) * 8, half * 64:(half + 1) * 64]
                if True:
                    P.op("act", lambda: nc.scalar.copy(out=o_, in_=blk3(psR, M)), r=[psR.res], w=[Xin.res])
                else:
                    P.op("dve", lambda: nc.vector.tensor_copy(out=o_, in_=blk3(psR, M)), r=[psR.res], w=[Xin.res])
            return ev

        def yin(c):
            return [Ybuf[:, 0, c, :], Ybuf[:, 1, c, :]]

        def yin_res(c):
            return YbR[c // 8]

        def xin_rows(K):
            return lambda c: [Xin[:K, c, :]]

        def xin_res(c):
            return Xin.res

        for g in range(4):
            short_conv(zhyT[g], g, vc, 8)
            short_conv(zhyT[4 + g], 4 + g, x1c, 8)
            short_conv(zhyT[8 + g], 8 + g, x2c, 2)
            for sub in range(2):
                cs = sub * 64
                c0abs = g * 128 + cs
                for hh_ in range(2):
                    P.dma("sp", dec0[hh_ * 64:(hh_ + 1) * 64, :], t_dec0[c0abs:c0abs + 64, :], w=[dec0.res])
                    P.dma("sp", decs[hh_ * 64:(hh_ + 1) * 64, :], t_decs[c0abs:c0abs + 64, :], w=[decs.res])
                for o in range(2):
                    filter_gen(c0abs, o)
                    to_xin_filter()
                    fft_pass(xin_rows(128), xin_res, 128, FAf, DFu, 128, True, evac_filt_first)
                    src = vc if o == 0 else y1
                    M = 64 if o == 0 else 16
                    to_xin(src, cs)
                    fft_pass(xin_rows(64), xin_res, 64, FAq, DFu, 128, True, evac_pointwise)
                    fft_pass(yin, yin_res, 128, FAu, DFq, M, False, make_evac_inv(M))
                    if o == 0:
                        for g16 in range(8):
                            ps = P.psum("tmp")
                            pbf = ps.t.bitcast(BF16)
                            for s_ in range(16):
                                lo = g16 * 16 + s_
                                P.op("pe", lambda: nc.tensor.transpose(out=pbf[cs:cs + 64, s_ * 64:(s_ + 1) * 64], in_=Xin[:64, :, lo],
                                                                       identity=ident[0:64, 0:64]), r=[Xin.res, ident.res], w=[ps.res])
                            gt_ = gt[0]
                            vw = lambda X_: X_[cs:cs + 64, :].rearrange("p (t l) -> p l t", l=128)[:, g16 * 16:(g16 + 1) * 16, :]
                            gv = gt_[cs:cs + 64, :].rearrange("p (l t) -> p l t", t=64)
                            P.op("dve", lambda: nc.vector.tensor_scalar(out=gv, in0=vw(vc), scalar1=skipc[cs:cs + 64, 0, g:g + 1], scalar2=None, op0=ALU.mult),
                                 r=[vc.res, skipc.res], w=[gt_.res])
                            P.op("dve", lambda: nc.vector.scalar_tensor_tensor(out=gv, in0=pbf[cs:cs + 64, 0:1024].rearrange("p (l t) -> p l t", t=64),
                                                                               scalar=rtot[cs:cs + 64, 0:1], in1=gv, op0=ALU.mult, op1=ALU.add),
                                 r=[ps.res, rtot.res, gt_.res], w=[gt_.res])
                            P.op("dve", lambda: nc.vector.tensor_tensor(out=vw(y1), in0=gv, in1=vw(x1c), op=ALU.mult),
                                 r=[gt_.res, x1c.res], w=[y1.res])
                    else:
                        for g64 in range(2):
                            ps = P.psum("tmp")
                            pbf = ps.t.bitcast(BF16)
                            for s_ in range(64):
                                lo = g64 * 64 + s_
                                P.op("pe", lambda: nc.tensor.transpose(out=pbf[cs:cs + 64, s_ * 16:(s_ + 1) * 16], in_=Xin[:16, :, lo],
                                                                       identity=ident[0:16, 0:16]), r=[Xin.res, ident.res], w=[ps.res])
                            gt_ = gt[0]
                            vw = lambda X_: X_[cs:cs + 64, 0:2048].rearrange("p (t l) -> p l t", l=128)[:, g64 * 64:(g64 + 1) * 64, :]
                            gv = gt_[cs:cs + 64, :].rearrange("p (l t) -> p l t", t=16)
                            P.op("dve", lambda: nc.vector.tensor_scalar(out=gv, in0=vw(y1), scalar1=skipc[cs:cs + 64, 1, g:g + 1], scalar2=None, op0=ALU.mult),
                                 r=[y1.res, skipc.res], w=[gt_.res])
                            P.op("dve", lambda: nc.vector.scalar_tensor_tensor(out=gv, in0=pbf[cs:cs + 64, 0:1024].rearrange("p (l t) -> p l t", t=16),
                                                                               scalar=rtot[cs:cs + 64, 1:2], in1=gv, op0=ALU.mult, op1=ALU.add),
                                 r=[ps.res, rtot.res, gt_.res], w=[gt_.res])
                            P.op("dve", lambda: nc.vector.tensor_tensor(out=vw(x2c), in0=gv, in1=vw(x2c), op=ALU.mult),
                                 r=[gt_.res, x2c.res], w=[x2c.res])
            P.dma("pool", hyTd[g].t.ap(), x2c[:], r=[x2c.res], w=[hyTd[g].res])
            if "hy1" in debug and g == 0:
                d1 = dbg_out("y1", [128, L], BF16)
                d2 = dbg_out("vc", [128, L], BF16)
                d3 = dbg_out("Kf", [128, 2 * 128 * CS], BF16)
                P.dma("sp", d1[:, :], y1[:], r=[y1.res], w=[d1.res])
                P.dma("sp", d2[:, :], vc[:], r=[vc.res], w=[d2.res])
                P.dma("sp", d3[:, :], Kf[:].rearrange("p r c k -> p (r c k)"), r=KfR, w=[d3.res])
            if "stopD1" in debug:
                break
        P.barrier()

    if "phaseD" in debug:
        d1 = dbg_out("hyT", [512, 2048], BF16)
        for g in range(4):
            P.dma("sp", d1[g * 128:(g + 1) * 128, :], hyTd[g].t.ap(), r=[hyTd[g].res], w=[d1.res])
    if "stopD" in debug or "stopD1" in debug:
        P.barrier()
        return nc, es, dbg

    x1d = P.dram("x1d", [2048, D], F32)
    h2T = P.sb(es, "h2T", [128, 8, 2048], BF16)
    with ExitStack() as pe_:
        whb = P.sb(pe_, "whb", [128, 4, D], BF16)
        wnb = P.sb(pe_, "wnb", [128, 4, D], BF16)
        wob = P.sb(pe_, "wob", [128, 8, D], BF16)
        with ExitStack() as pst:
            wstg = P.sb(pst, "wstg", [128, 4, D], F32)
            P.dma("sp", wstg[:], w_hy_out[:, :].rearrange("(kc p) n -> p kc n", p=128), w=[wstg.res])
            P.op("dve", lambda: nc.vector.tensor_copy(out=whb[:], in_=wstg[:]), r=[wstg.res], w=[whb.res])
            P.dma("sp", wstg[:], w_na_out[:, :].rearrange("(kc p) n -> p kc n", p=128), w=[wstg.res])
            P.op("pool", lambda: nc.gpsimd.tensor_copy(out=wnb[:], in_=wstg[:]), r=[wstg.res], w=[wnb.res])
            for hh in range(2):
                P.dma("sp", wstg[:], w_out[hh * 512:(hh + 1) * 512, :].rearrange("(kc p) n -> p kc n", p=128), w=[wstg.res])
                P.op("dve", lambda: nc.vector.tensor_copy(out=wob[:, hh * 4:(hh + 1) * 4, :], in_=wstg[:]), r=[wstg.res], w=[wob.res])
            P.barrier()
        hyb = P.sb(pe_, "hyb", [128, 4, 512], BF16)
        atb = P.sb(pe_, "atb", [128, 4, 512], BF16)
        gtb = P.sb(pe_, "gtb", [128, 16, 512], BF16)
        mrg = P.sb(pe_, "mrg", [128, 8, 512], BF16)
        m1 = [P.sb(pe_, "m1_%d" % i, [128, 512], F32) for i in range(2)]
        m2 = [P.sb(pe_, "m2_%d" % i, [128, 512], F32) for i in range(2)]
        xe = [P.sb(pe_, "xe%d" % i, [128, D], F32) for i in range(2)]
        x1t = [P.sb(pe_, "x1t%d" % i, [128, D], F32) for i in range(2)]
        xn2 = [P.sb(pe_, "xn2_%d" % i, [128, D], BF16) for i in range(2)]
        junk2 = P.sb(pe_, "junk2", [128, D], BF16)
        se = [P.sb(pe_, "se%d" % i, [128, 4], F32) for i in range(2)]
        for blk in range(4):
            tcols = slice(blk * 512, (blk + 1) * 512)
            for g in range(4):
                P.dma("sp", hyb[:, g, :], hyTd[g][:, tcols], r=[hyTd[g].res], w=[hyb.res])
            P.dma("sp", atb[:], attTd[:, :, tcols], r=[attTd.res], w=[atb.res])
            for m in range(16):
                P.dma("sp", gtb[:, m, :], gsT[m][:, tcols], r=[gsT[m].res], w=[gtb.res])
            for fc in range(8):
                fs = slice(fc * 128, (fc + 1) * 128)
                psH = P.psum("acc")
                psA = P.psum("acc")
                for kc in range(4):
                    P.op("pe", lambda: nc.tensor.matmul(psH[:, :], lhsT=whb[:, kc, fs], rhs=hyb[:, kc, :], start=(kc == 0), stop=(kc == 3)),
                         r=[whb.res, hyb.res], w=[psH.res])
                for kc in range(4):
                    P.op("pe", lambda: nc.tensor.matmul(psA[:, :], lhsT=wnb[:, kc, fs], rhs=atb[:, kc, :], start=(kc == 0), stop=(kc == 3)),
                         r=[wnb.res, atb.res], w=[psA.res])
                a_, b_ = m1[fc % 2], m2[fc % 2]
                P.op("dve", lambda: nc.vector.tensor_tensor(out=a_[:], in0=psH[:, :], in1=gtb[:, fc, :], op=ALU.mult), r=[psH.res, gtb.res], w=[a_.res])
                P.op("dve", lambda: nc.vector.tensor_tensor(out=b_[:], in0=psA[:, :], in1=gtb[:, 8 + fc, :], op=ALU.mult), r=[psA.res, gtb.res], w=[b_.res])
                P.op("pool", lambda: nc.gpsimd.tensor_tensor(out=mrg[:, fc, :], in0=a_[:], in1=b_[:], op=ALU.add), r=[a_.res, b_.res], w=[mrg.res])
            for tt in range(4):
                i = blk * 4 + tt
                x_ = xe[i % 2]
                x1_ = x1t[i % 2]
                xn_ = xn2[i % 2]
                s_ = se[i % 2]
                P.dma("sp", x_[:], xroll[i * 128:(i + 1) * 128, :], w=[x_.res])
                for hh in range(2):
                    hs = slice(hh * 512, (hh + 1) * 512)
                    ps = P.psum("tmp")
                    for fc in range(8):
                        P.op("pe", lambda: nc.tensor.matmul(ps[:, :], lhsT=mrg[:, fc, tt * 128:(tt + 1) * 128], rhs=wob[:, fc, hs],
                                                            start=(fc == 0), stop=(fc == 7)), r=[mrg.res, wob.res], w=[ps.res])
                    P.op("dve", lambda: nc.vector.tensor_tensor(out=x1_[:, hs], in0=ps[:, :], in1=grow[:, 0, hs], op=ALU.mult),
                         r=[ps.res, grow.res], w=[x1_.res])
                    P.op("pool", lambda: nc.gpsimd.tensor_tensor(out=x1_[:, hs], in0=x1_[:, hs], in1=x_[:, hs], op=ALU.add),
                         r=[x1_.res, x_.res], w=[x1_.res])
                P.dma("pool", x1d[i * 128:(i + 1) * 128, :], x1_[:], r=[x1_.res], w=[x1d.res])
                P.op("act", lambda: nc.scalar.activation(out=junk2[:], in_=x1_[:], func=AF.Square, accum_out=s_[:, 0:1]),
                     r=[x1_.res], w=[junk2.res, s_.res])
                P.op("dve", lambda: nc.vector.tensor_scalar(out=s_[:, 1:2], in0=s_[:, 0:1], scalar1=1.0 / D, scalar2=EPS,
                                                            op0=ALU.mult, op1=ALU.add), r=[s_.res], w=[s_.res])
                P.op("act", lambda: nc.scalar.activation(out=s_[:, 2:3], in_=s_[:, 1:2], func=AF.Sqrt), r=[s_.res], w=[s_.res])
                P.op("dve", lambda: nc.vector.reciprocal(out=s_[:, 3:4], in_=s_[:, 2:3]), r=[s_.res], w=[s_.res])
                P.op("dve", lambda: nc.vector.tensor_scalar(out=xn_[:], in0=x1_[:], scalar1=s_[:, 3:4], scalar2=None, op0=ALU.mult),
                     r=[x1_.res, s_.res], w=[xn_.res])
                pb_ = P.psum("tmp")
                pbf = pb_.t.bitcast(BF16)
                for kc in range(8):
                    P.op("pe", lambda: nc.tensor.transpose(out=pbf[:, kc * 128:(kc + 1) * 128], in_=xn_[:, kc * 128:(kc + 1) * 128],
                                                           identity=ident[:]), r=[xn_.res, ident.res], w=[pb_.res])
                for kc in range(8):
                    P.op("act", lambda: nc.scalar.activation(out=h2T[:, kc, i * 128:(i + 1) * 128], in_=pbf[:, kc * 128:(kc + 1) * 128],
                                                             func=AF.Identity, scale=mods[:, MI_G2, kc:kc + 1], bias=mods[:, MI_S2, kc:kc + 1]),
                         r=[pb_.res, mods.res], w=[h2T.res])
        P.barrier()

    if "phaseE" in debug:
        d1 = dbg_out("x1", [2048, D])
        d2 = dbg_out("h2T", [128, 8 * 2048], BF16)
        P.dma("sp", d1[:, :], x1d.t.ap(), r=[x1d.res], w=[d1.res])
        P.dma("sp", d2[:, :], h2T[:].rearrange("p k t -> p (k t)"), r=[h2T.res], w=[d2.res])
    if "stopE" in debug:
        P.barrier()
        return nc, es, dbg

    I1T = P.sb(es, "I1T", [128, 2048], F32)
    I2T = P.sb(es, "I2T", [128, 2048], F32)
    GWT = P.sb(es, "GWT", [128, 2048], F32)
    io128 = P.sb(es, "io128", [128, 128], F32)
    io16 = P.sb(es, "io16", [128, 16], F32)
    P.dma("sp", io128[:], t_io128[:, :], w=[io128.res])
    P.dma("sp", io16[:], t_io128[:, 0:16], w=[io16.res])
    with ExitStack() as pf1:
        wqb = P.sb(pf1, "wqb", [128, 8, 2048], BF16)
        wqs = [P.sb(pf1, "wqs%d" % i, [128, 8, 256], F32) for i in range(2)]
        keyb = P.sb(pf1, "keyb", [128, 16, 128], BF16)
        keyf = P.sb(pf1, "keyf", [128, 16, 128], F32)
        P.dma("sp", keyf[:], peer_keysT[:, :, :], w=[keyf.res])
        P.op("pool", lambda: nc.gpsimd.tensor_copy(out=keyb[:], in_=keyf[:]), r=[keyf.res], w=[keyb.res])
        for pc in range(8):
            ws = wqs[pc % 2]
            P.dma("sp", ws[:], peer_w_q[:, pc * 256:(pc + 1) * 256].rearrange("(kc p) n -> p kc n", p=128), w=[ws.res])
            eng = "dve" if pc % 2 == 0 else "pool"
            P.op(eng, lambda: P.engs[eng].tensor_copy(out=wqb[:, :, pc * 256:(pc + 1) * 256], in_=ws[:]), r=[ws.res], w=[wqb.res])
        qpT = P.sb(pf1, "qpT", [128, 16, 512], BF16)
        sc = P.sb(pf1, "sc", [128, 16, 128], F32)
        sc2 = P.sb(pf1, "sc2", [128, 16, 128], F32)
        top = P.sb(pf1, "top", [128, 16, 16], F32)
        idxu = P.sb(pf1, "idxu", [128, 16, 16], U32)
        idxf = P.sb(pf1, "idxf", [128, 16, 16], F32)
        cand = P.sb(pf1, "cand", [128, 8, 256], F32)
        cand2 = P.sb(pf1, "cand2", [128, 8, 256], F32)
        best = P.sb(pf1, "best", [128, 8, 16], F32)
        posu = P.sb(pf1, "posu", [128, 8, 16], U32)
        pos2 = P.sb(pf1, "pos2", [128, 2, 128], U32)
        posf = P.sb(pf1, "posf", [128, 2, 128], F32)
        eq = P.sb(pf1, "eq", [128, 128, 16], F32)
        sm = P.sb(pf1, "sm", [128, 4, 128], F32)
        ssum = P.sb(pf1, "ssum", [128, 2, 8], F32)
        for blk in range(4):
            tcols = slice(blk * 512, (blk + 1) * 512)
            for hp in range(16):
                ps = P.psum("tmp")
                for kc in range(8):
                    P.op("pe", lambda: nc.tensor.matmul(ps[:, :], lhsT=wqb[:, kc, hp * 128:(hp + 1) * 128], rhs=h2T[:, kc, tcols],
                                                        start=(kc == 0), stop=(kc == 7)), r=[wqb.res, h2T.res], w=[ps.res])
                if hp % 2 == 0:
                    P.op("act", lambda: nc.scalar.copy(out=qpT[:, hp, :], in_=ps[:, :]), r=[ps.res], w=[qpT.res])
                else:
                    P.op("dve", lambda: nc.vector.tensor_copy(out=qpT[:, hp, :], in_=ps[:, :]), r=[ps.res], w=[qpT.res])
            for tt in range(4):
                i = blk * 4 + tt
                for h4 in range(4):
                    ps = P.psum("tmp")
                    for j in range(4):
                        hp = h4 * 4 + j
                        P.op("pe", lambda: nc.tensor.matmul(ps[:, j * 128:(j + 1) * 128], lhsT=qpT[:, hp, tt * 128:(tt + 1) * 128],
                                                            rhs=keyb[:, hp, :], start=True, stop=True), r=[qpT.res, keyb.res], w=[ps.res])
                    P.op("act", lambda: nc.scalar.copy(out=sc[:, h4 * 4:(h4 + 1) * 4, :], in_=ps[:, :].rearrange("p (j n) -> p j n", n=128)),
                         r=[ps.res], w=[sc.res])
                for hp in range(16):
                    P.op("dve", lambda: nc.vector.max(out=top[:, hp, 0:8], in_=sc[:, hp, :]), r=[sc.res], w=[top.res])
                    P.op("dve", lambda: nc.vector.max_index(out=idxu[:, hp, 0:8], in_max=top[:, hp, 0:8], in_values=sc[:, hp, :]),
                         r=[sc.res, top.res], w=[idxu.res])
                    P.op("dve", lambda: nc.vector.match_replace(out=sc2[:, hp, :], in_to_replace=top[:, hp, 0:8], in_values=sc[:, hp, :],
                                                                imm_value=-1e30), r=[sc.res, top.res], w=[sc2.res])
                    P.op("dve", lambda: nc.vector.max(out=top[:, hp, 8:16], in_=sc2[:, hp, :]), r=[sc2.res], w=[top.res])
                    P.op("dve", lambda: nc.vector.max_index(out=idxu[:, hp, 8:16], in_max=top[:, hp, 8:16], in_values=sc2[:, hp, :]),
                         r=[sc2.res, top.res], w=[idxu.res])
                P.op("dve", lambda: nc.vector.tensor_copy(out=idxf[:], in_=idxu[:]), r=[idxu.res], w=[idxf.res])
                top4 = top[:].rearrange("p (h t) k -> p h t k", t=2)
                idx4 = idxf[:].rearrange("p (h t) k -> p h t k", t=2)
                P.op("dve", lambda: nc.vector.tensor_tensor(out=cand[:].rearrange("p h (a b) -> p h a b", b=16),
                                                            in0=top4[:, :, 0, :].unsqueeze(3).to_broadcast([128, 8, 16, 16]),
                                                            in1=top4[:, :, 1, :].unsqueeze(2).to_broadcast([128, 8, 16, 16]), op=ALU.add),
                     r=[top.res], w=[cand.res])
                for h in range(8):
                    P.op("dve", lambda: nc.vector.max(out=best[:, h, 0:8], in_=cand[:, h, :]), r=[cand.res], w=[best.res])
                    P.op("dve", lambda: nc.vector.max_index(out=posu[:, h, 0:8], in_max=best[:, h, 0:8], in_values=cand[:, h, :]),
                         r=[cand.res, best.res], w=[posu.res])
                    P.op("dve", lambda: nc.vector.match_replace(out=cand2[:, h, :], in_to_replace=best[:, h, 0:8], in_values=cand[:, h, :],
                                                                imm_value=-1e30), r=[cand.res, best.res], w=[cand2.res])
                    P.op("dve", lambda: nc.vector.max(out=best[:, h, 8:16], in_=cand2[:, h, :]), r=[cand2.res], w=[best.res])
                    P.op("dve", lambda: nc.vector.max_index(out=posu[:, h, 8:16], in_max=best[:, h, 8:16], in_values=cand2[:, h, :]),
                         r=[cand2.res, best.res], w=[posu.res])
                sm3 = lambda k_: sm[:, k_, :].rearrange("p (h k) -> p h k", k=16)
                P.op("dve", lambda: nc.vector.tensor_tensor(out=sm3(0), in0=best[:], in1=best[:, :, 0:1].to_broadcast([128, 8, 16]), op=ALU.subtract),
                     r=[best.res], w=[sm.res])
                P.op("act", lambda: nc.scalar.activation(out=sm[:, 0, :], in_=sm[:, 0, :], func=AF.Exp), r=[sm.res], w=[sm.res])
                P.op("dve", lambda: nc.vector.tensor_reduce(out=ssum[:, 0, :], in_=sm3(0), axis=AX.X, op=ALU.add), r=[sm.res], w=[ssum.res])
                P.op("dve", lambda: nc.vector.reciprocal(out=ssum[:, 1, :], in_=ssum[:, 0, :]), r=[ssum.res], w=[ssum.res])
                P.op("dve", lambda: nc.vector.tensor_tensor(out=sm3(1), in0=sm3(0), in1=ssum[:, 1, :].unsqueeze(2).to_broadcast([128, 8, 16]), op=ALU.mult),
                     r=[sm.res, ssum.res], w=[sm.res])
                posv = posu[:].rearrange("p h k -> p (h k)")
                P.op("dve", lambda: nc.vector.tensor_single_scalar(out=pos2[:, 0, :], in_=posv, scalar=4, op=ALU.logical_shift_right),
                     r=[posu.res], w=[pos2.res])
                P.op("dve", lambda: nc.vector.tensor_single_scalar(out=pos2[:, 1, :], in_=posv, scalar=15, op=ALU.bitwise_and),
                     r=[posu.res], w=[pos2.res])
                P.op("dve", lambda: nc.vector.tensor_copy(out=posf[:], in_=pos2[:]), r=[pos2.res], w=[posf.res])
                for t_ in range(2):
                    P.op("dve", lambda: nc.vector.tensor_tensor(out=eq[:], in0=posf[:, t_, :].unsqueeze(2).to_broadcast([128, 128, 16]),
                                                                in1=io16[:].unsqueeze(1).to_broadcast([128, 128, 16]), op=ALU.is_equal),
                         r=[posf.res, io16.res], w=[eq.res])
                    eq4 = eq[:].rearrange("p (h k) c -> p h k c", k=16)
                    P.op("dve", lambda: nc.vector.tensor_tensor(out=eq4, in0=eq4, in1=idx4[:, :, t_, :].unsqueeze(2).to_broadcast([128, 8, 16, 16]),
                                                                op=ALU.mult), r=[eq.res, idxf.res], w=[eq.res])
                    P.op("dve", lambda: nc.vector.tensor_reduce(out=sm[:, 2 + t_, :], in_=eq[:], axis=AX.X, op=ALU.add), r=[eq.res], w=[sm.res])
                for k_, dstT in ((2, I1T), (3, I2T), (1, GWT)):
                    ps = P.psum("tmp")
                    P.op("pe", lambda: nc.tensor.transpose(out=ps[:, 0:128], in_=sm[:, k_, :], identity=identf[:]), r=[sm.res, identf.res], w=[ps.res])
                    P.op("act", lambda: nc.scalar.copy(out=dstT[:, i * 128:(i + 1) * 128], in_=ps[:, 0:128]), r=[ps.res], w=[dstT.res])
        P.barrier()

    if "phaseF1" in debug:
        d1 = dbg_out("I1T", [128, 2048])
        d2 = dbg_out("I2T", [128, 2048])
        d3 = dbg_out("GWT", [128, 2048])
        P.dma("sp", d1[:, :], I1T[:], r=[I1T.res], w=[d1.res])
        P.dma("sp", d2[:, :], I2T[:], r=[I2T.res], w=[d2.res])
        P.dma("sp", d3[:, :], GWT[:], r=[GWT.res], w=[d3.res])
    if "stopF1" in debug:
        P.barrier()
        return nc, es, dbg

    with ExitStack() as pf2:
        GTb = P.sb(pf2, "GTb", [128, 128, 256], BF16)
        TC = 16
        O2 = P.sb(pf2, "O2", [128, TC, 128], BF16)
        O1e = P.sb(pf2, "O1e", [128, TC, 128], BF16)
        O1 = P.sb(pf2, "O1", [128, TC, 128], BF16)
        ubt = [P.sb(pf2, "ubt%d" % i, [128, 4, 8, 128], BF16) for i in range(3)]
        vbt = [P.sb(pf2, "vbt%d" % i, [128, 4, D], BF16) for i in range(3)]
        Ag = [P.sb(pf2, "Ag%d" % i, [128, 256], BF16) for i in range(3)]
        AG = [P.sb(pf2, "AG%d" % i, [128, 256], BF16) for i in range(3)]
        x1f = [P.sb(pf2, "x1f%d" % i, [128, D], F32) for i in range(1)]
        of_ = [P.sb(pf2, "of%d" % i, [128, D], F32) for i in range(2)]
        for b8 in range(8):
            t0 = b8 * 256
            for ch in range(256 // TC):
                c0 = t0 + ch * TC
                P.op("dve", lambda: nc.vector.tensor_tensor(out=O2[:], in0=io128[:].unsqueeze(1).to_broadcast([128, TC, 128]),
                                                            in1=I2T[:, c0:c0 + TC].unsqueeze(2).to_broadcast([128, TC, 128]), op=ALU.is_equal),
                     r=[io128.res, I2T.res], w=[O2.res])
                P.op("dve", lambda: nc.vector.tensor_tensor(out=O1e[:], in0=io128[:].unsqueeze(1).to_broadcast([128, TC, 128]),
                                                            in1=I1T[:, c0:c0 + TC].unsqueeze(2).to_broadcast([128, TC, 128]), op=ALU.is_equal),
                     r=[io128.res, I1T.res], w=[O1e.res])
                P.op("pool", lambda: nc.gpsimd.tensor_tensor(out=O1[:], in0=O1e[:], in1=GWT[:, c0:c0 + TC].unsqueeze(2).to_broadcast([128, TC, 128]),
                                                             op=ALU.mult), r=[O1e.res, GWT.res], w=[O1.res])
                for t4 in range(TC // 4):
                    ps = P.psum("tmp")
                    for j in range(4):
                        t_ = t4 * 4 + j
                        P.op("pe", lambda: nc.tensor.matmul(ps[:, j * 128:(j + 1) * 128], lhsT=O1[:, t_, :], rhs=O2[:, t_, :], start=True, stop=True),
                             r=[O1.res, O2.res], w=[ps.res])
                    o_ = GTb[:, :, ch * TC + t4 * 4:ch * TC + t4 * 4 + 4]
                    i_ = ps[:, :].rearrange("p (t j) -> p j t", j=128)
                    if t4 % 2 == 0:
                        P.op("act", lambda: nc.scalar.copy(out=o_, in_=i_), r=[ps.res], w=[GTb.res])
                    else:
                        P.op("dve", lambda: nc.vector.tensor_copy(out=o_, in_=i_), r=[ps.res], w=[GTb.res])
            accs = [[P.psum("acc") for _ in range(2)] for _ in range(2)]
            def s_stage(jp):
                u_ = ubt[(jp // 4) % 3]
                v_ = vbt[(jp // 4) % 3]
                j4 = jp % 4
                if j4 == 0:
                    P.dma("sp", u_[:], ubd[jp // 4, :, :, :, :], r=[ubd_res], w=[u_.res])
                    P.dma("sp", v_[:], vbd[jp // 4, :, :, :], r=[vbd_res], w=[v_.res])
                psS = P.psum("tmp")
                for kc in range(8):
                    P.op("pe", lambda: nc.tensor.matmul(psS[:, 0:256], lhsT=u_[:, j4, kc, :], rhs=h2T[:, kc, t0:t0 + 256],
                                                        start=(kc == 0), stop=(kc == 7)), r=[u_.res, h2T.res], w=[psS.res])
                a_ = Ag[jp % 3]
                g_ = AG[jp % 3]
                P.op("act", lambda: nc.scalar.activation(out=a_[:], in_=psS[:, 0:256], func=AF.Gelu), r=[psS.res], w=[a_.res])
                P.op("dve", lambda: nc.vector.tensor_tensor(out=g_[:], in0=a_[:], in1=GTb[:, jp, :], op=ALU.mult), r=[a_.res, GTb.res], w=[g_.res])
                return g_, v_, j4

            def acc_stage(jp, st_):
                g_, v_, j4 = st_
                for tt in range(2):
                    for hh in range(2):
                        P.op("pe", lambda: nc.tensor.matmul(accs[tt][hh][:, :], lhsT=g_[:, tt * 128:(tt + 1) * 128], rhs=v_[:, j4, hh * 512:(hh + 1) * 512],
                                                            start=(jp == 0), stop=(jp == 127)), r=[g_.res, v_.res], w=[accs[tt][hh].res])

            prev = None
            for jp in range(128):
                cur = s_stage(jp)
                if prev is not None:
                    acc_stage(jp - 1, prev)
                prev = cur
            acc_stage(127, prev)
            for tt in range(2):
                i = b8 * 2 + tt
                xf = x1f[0]
                o_ = of_[tt]
                P.dma("sp", xf[:], x1d[i * 128:(i + 1) * 128, :], r=[x1d.res], w=[xf.res])
                for hh in range(2):
                    hs = slice(hh * 512, (hh + 1) * 512)
                    P.op("dve", lambda: nc.vector.tensor_tensor(out=o_[:, hs], in0=accs[tt][hh][:, :], in1=grow[:, 1, hs], op=ALU.mult),
                         r=[accs[tt][hh].res, grow.res], w=[o_.res])
                    P.op("pool", lambda: nc.gpsimd.tensor_tensor(out=o_[:, hs], in0=o_[:, hs], in1=xf[:, hs], op=ALU.add),
                         r=[o_.res, xf.res], w=[o_.res])
                P.dma("pool", out[i * 128:(i + 1) * 128, :], o_[:], r=[o_.res], w=[out.res])
        P.barrier()

    P.barrier()
    return nc, es, dbg


def _colT(v, nch):
    return np.ascontiguousarray(np.asarray(v, np.float32).reshape(nch, 128).T)


def prep_inputs(inp):
    f = lambda a: np.ascontiguousarray(np.asarray(a, np.float32))
    x, c, ctx, c_ctx = f(inp["x"]), f(inp["c"]), f(inp["ctx"]), f(inp["c_ctx"])
    w_ada = f(inp["w_ada"][0])
    b_ada = f(inp["b_ada"][0])
    shared = {
        "w_ada": w_ada,
        "b_adaT": _colT(b_ada, 48),
        "b_ada_g": np.ascontiguousarray(np.stack([b_ada[2 * D:3 * D], b_ada[5 * D:6 * D]])),
        "n1T": _colT(inp["norm1_g"][0], 8),
        "n2T": _colT(inp["norm2_g"][0], 8),
        "w_in": f(inp["w_in"][0]),
        "b_inT": _colT(inp["b_in"][0], 40),
        "b_in_qkv": f(inp["b_in"][0][1536:3072]).reshape(1, 1536),
        "ident": np.eye(128, dtype=np.float32),
        "peer_w_q": f(inp["peer_w_q"][0]),
        "peer_keysT": np.ascontiguousarray(f(inp["peer_keys"][0]).reshape(16, 128, 128).transpose(2, 0, 1)),
        "peer_uTp": np.ascontiguousarray(f(inp["peer_u"][0]).reshape(128, 128, D).transpose(2, 1, 0)),
        "peer_v": f(inp["peer_v"][0]),
        "t_io128": np.ascontiguousarray(np.broadcast_to(np.arange(128, dtype=np.float32)[None, :], (128, 128))),
        "w_hy_out": f(inp["w_hy_out"][0]),
        "w_na_out": f(inp["w_na_out"][0]),
        "w_out": f(inp["w_out"][0]),
        "q_norm_g": f(inp["q_norm_g"][0]).reshape(1, 64),
        "k_norm_g": f(inp["k_norm_g"][0]).reshape(1, 64),
    }
    import ml_dtypes
    bf16 = ml_dtypes.bfloat16
    ar128 = np.arange(128, dtype=np.float64)
    ang = 2.0 * np.pi * np.outer(ar128, ar128) / 128.0
    Fr, Fi = np.cos(ang), -np.sin(ang)

    def fa_tab(rows):
        t = np.zeros((len(rows), 2, 2, 2, 2, 64))
        for hf_ in range(2):
            ks = slice(hf_ * 64, (hf_ + 1) * 64)
            t[:, 0, 0, hf_, 0], t[:, 0, 0, hf_, 1] = Fr[rows][:, ks], Fi[rows][:, ks]
            t[:, 0, 1, hf_, 0], t[:, 0, 1, hf_, 1] = -Fi[rows][:, ks], Fr[rows][:, ks]
        t[:, 1, :, :, 0], t[:, 1, :, :, 1] = t[:, 0, :, :, 1], t[:, 0, :, :, 0]
        return np.ascontiguousarray(t.reshape(len(rows), 2, 2, 2, 128).astype(np.float32).astype(bf16))

    def df_tab(cols):
        t = np.zeros((128, 3, len(cols)))
        t[:, 0], t[:, 1], t[:, 2] = Fr[:, cols], Fi[:, cols], -Fi[:, cols]
        return np.ascontiguousarray(t.astype(np.float32).astype(bf16))

    angt = 2.0 * np.pi * np.outer(ar128, ar128) / 16384.0
    shared["t_FAu"] = fa_tab(np.arange(128))
    shared["t_FAf"] = fa_tab(np.concatenate([np.arange(64), 127 - np.arange(64)]))
    shared["t_DFu"] = df_tab(np.arange(128))
    twr_, twi_ = np.cos(angt), -np.sin(angt)
    twt = np.zeros((128, 2, 2, 2, 64))
    for hf_ in range(2):
        ks = slice(hf_ * 64, (hf_ + 1) * 64)
        twt[:, 0, hf_, 0], twt[:, 0, hf_, 1] = twr_[:, ks], twr_[:, ks]
        twt[:, 1, hf_, 0], twt[:, 1, hf_, 1] = twi_[:, ks], twi_[:, ks]
    shared["t_twb"] = np.ascontiguousarray(twt.reshape(128, 2, 2, 128).astype(np.float32).astype(bf16))
    shared["t_sel"] = np.ascontiguousarray((np.arange(128)[:, None] % 64 == np.arange(128)[None, :] % 64).astype(np.float32))
    tl = np.linspace(0.0, 1.0, L, dtype=np.float32)[:, None]
    wl = (np.float32(2.0 * math.pi / L) * np.arange(L, dtype=np.float32))[:, None]
    bands = np.linspace(1e-4, 15.0, 16, dtype=np.float32)[None, :]
    shared["t_featsT"] = np.ascontiguousarray(np.concatenate([tl, np.cos(bands * wl), -np.sin(bands * wl)], axis=-1).T.astype(np.float32))
    deltas = np.abs(np.linspace(math.log(1e-2) / 1.5, math.log(1e-2) / 0.3, 512, dtype=np.float32))
    i512 = (np.arange(512, dtype=np.float64) / (L - 1))[None, :]
    b16 = (512.0 * np.arange(16, dtype=np.float64) / (L - 1))[None, :]
    shared["t_dec0"] = np.ascontiguousarray(np.exp(-deltas.astype(np.float64)[:, None] * i512).astype(np.float32))
    shared["t_decs"] = np.ascontiguousarray(np.exp(-deltas.astype(np.float64)[:, None] * b16).astype(np.float32))
    cw = f(inp["hy_conv_w"][0])
    shared["hy_convw"] = np.ascontiguousarray(cw.reshape(3, 12, 128).transpose(2, 1, 0))
    shared["hy_convb"] = _colT(inp["hy_conv_b"][0], 12)
    shared["hy_skipc"] = np.ascontiguousarray(f(inp["hy_skip"][0]).reshape(2, 4, 128).transpose(2, 0, 1))
    shared["hy_f1_w"] = f(inp["hy_f1_w"][0])
    shared["hy_f2_w"] = f(inp["hy_f2_w"][0])
    shared["hy_f3_w"] = f(inp["hy_f3_w"][0])
    shared["hy_f4_w"] = f(inp["hy_f4_w"][0])
    shared["hy_fbc"] = np.ascontiguousarray(np.stack([f(inp["hy_f1_b"][0]), f(inp["hy_f2_b"][0]), f(inp["hy_f3_b"][0]),
                                                      f(inp["hy_sin_freq"][0])], axis=1))
    inv = (10000.0 ** (-np.arange(16, dtype=np.float32) / np.float32(16))).astype(np.float32)
    rpb = f(inp["na_rpb"][0])
    kl = np.arange(2)[:, None, None, None, None]
    ck = np.arange(64)[None, :, None, None, None]
    cc = np.arange(8)[None, None, :, None, None]
    qr_ = np.arange(8)[None, None, None, :, None]
    cq = np.arange(64)[None, None, None, None, :]
    dr = 2 * cc + kl - qr_ + 3
    dc = np.clip(ck - cq, -15, 15) + 15
    drv = (dr >= 0) & (dr <= 14)
    bx = rpb[:, np.clip(dr, 0, 14), dc]
    bx = np.where(np.broadcast_to(drv, bx.shape[1:])[None], bx, np.float32(0.0))
    shared["biasx"] = np.ascontiguousarray(bx.reshape(8, 128, 8, 512).astype(np.float32))
    maps = []
    for core in range(N_CORES):
        b, q = core // 4, core % 4
        m = dict(shared)
        m["xroll"] = np.ascontiguousarray(np.roll(x[b], -2048 * q, axis=0))
        m["cT"] = np.ascontiguousarray(np.stack([_colT(c[b], 8), _colT(c_ctx, 8)], axis=-1))
        m["ctxb"] = np.ascontiguousarray(ctx[b])
        tiles = np.arange(64)
        hi_of = np.where(tiles < 64 - 16 * q, tiles, tiles + 64)
        m["t_FAq"] = fa_tab(hi_of)
        m["t_DFq"] = df_tab(hi_of)
        seam = (L - 2048 * q) % L
        fl = np.array([1.0 if 2048 * j == seam else 0.0 for j in range(4)], np.float32)
        omf = np.stack([1.0 - fl, 1.0 - np.roll(fl, -1)], axis=0)
        m["t_omf"] = np.ascontiguousarray(np.broadcast_to(omf[None], (128, 2, 4)).astype(np.float32))
        jj = np.arange(4)[:, None, None, None, None, None]
        rk = 32 * q + 8 * jj + 2 * cc[None] - 4 + kl[None]
        rq = 32 * q + 8 * jj + qr_[None]
        rs = np.clip(rq - 4, 0, 120)
        cs = np.clip(cq[None] - 8, 0, 48)
        vis = (rk >= rs) & (rk < rs + 8) & (rk >= 0) & (rk < 128) & (ck[None] >= cs) & (ck[None] < cs + 16)
        m["negmask"] = np.ascontiguousarray(np.where(vis, np.float32(0.0), np.float32(-30000.0)).reshape(4, 128, 8, 512).astype(np.float32))
        pos = (np.arange(20 * 128) - 256 + 2048 * q) % L
        ar = (pos // 64).astype(np.float32)[:, None] * inv[None, :]
        ac = (pos % 64).astype(np.float32)[:, None] * inv[None, :]
        m["rope"] = np.ascontiguousarray(np.concatenate([np.cos(ar), np.cos(ac), np.sin(ar), np.sin(ac)], axis=1).astype(np.float32))
        maps.append(m)
    return maps


def kernel(**inputs):
    nc, es, dbg = build_program()
    maps = prep_inputs(inputs)
    res = run_bass_kernel_spmd(nc, maps, core_ids=list(range(N_CORES)))
    es.close()
    outp = np.zeros((2, L, D), np.float32)
    for core in range(N_CORES):
        b, q = core // 4, core % 4
        outp[b, 2048 * q:2048 * (q + 1)] = res.results[core]["out"]
    return outp
```

```python
import math
from contextlib import ExitStack

import numpy as np
import concourse.bass as bass
import concourse.mybir as mybir
from concourse.bass_utils import run_bass_kernel_spmd

F32 = mybir.dt.float32
BF16 = mybir.dt.bfloat16
U32 = mybir.dt.uint32
I32 = mybir.dt.int32
AF = mybir.ActivationFunctionType
ALU = mybir.AluOpType
AX = mybir.AxisListType

D = 1024
L = 8192
NPROJ = 5120
EPS = 1e-6
N_CORES = 8


class Res:
    __slots__ = ("name", "w", "r", "subs")

    def __init__(self, name="", subs=None):
        self.name = name
        self.w = None
        self.r = {}
        self.subs = subs


def _expand(lst):
    out = []
    for x in lst:
        if x.subs is not None:
            out.extend(x.subs)
        else:
            out.append(x)
    return out


class T:
    def __init__(self, t, name):
        self.t = t
        self.res = Res(name)

    def __getitem__(self, idx):
        return self.t[idx]


class Prog:
    EPOCH = 12000

    def __init__(self, nc, es):
        self.nc = nc
        self.es = es
        self.S = []
        self.engs = {"pe": nc.tensor, "act": nc.scalar, "dve": nc.vector, "pool": nc.gpsimd, "sp": nc.sync}
        self.cur = {}
        self.cnt = {}
        self.last = {}
        self.pe_sids = set()
        for e in ("pe", "act", "dve", "pool"):
            self.cur[e] = self._sem(e + "0")
            self.cnt[e] = 0
            self.last[e] = None
        self.pe_sids.add(self.cur["pe"])
        self.waited = {e: {} for e in self.engs}
        self.dq = {}
        self.dqi = {}
        for q, n in (("sp", 16), ("pool", 12), ("act", 6)):
            self.dq[q] = [[self._sem("d%s%d" % (q, i)), 0] for i in range(n)]
            self.dqi[q] = 0
        self.ninst = 0
        self._ps = None
        self._psi = 0
        self._psg = {}

    def _sem(self, name):
        h = self.es.enter_context(self.nc.semaphore(name))
        self.S.append(h)
        return len(self.S) - 1

    def wait(self, e, ev):
        sid, val = ev
        if self.waited[e].get(sid, 0) >= val:
            return
        self.engs[e].wait_ge(self.S[sid], val)
        self.waited[e][sid] = val

    @staticmethod
    def _deps(r, w):
        r, w = _expand(r), _expand(w)
        deps = {}
        for x in r:
            if x.w is not None and deps.get(x.w[0], 0) < x.w[1]:
                deps[x.w[0]] = x.w[1]
        for x in w:
            if x.w is not None and deps.get(x.w[0], 0) < x.w[1]:
                deps[x.w[0]] = x.w[1]
            for sid, val in x.r.items():
                if deps.get(sid, 0) < val:
                    deps[sid] = val
        return deps

    @staticmethod
    def _mark(ev, r, w):
        r, w = _expand(r), _expand(w)
        sid, val = ev
        for x in r:
            if x.r.get(sid, 0) < val:
                x.r[sid] = val
        for x in w:
            x.w = ev
            x.r = {}

    def op(self, e, fn, r=(), w=()):
        for sid, val in self._deps(r, w).items():
            if e == "pe" and sid in self.pe_sids:
                continue
            self.wait(e, (sid, val))
        inst = fn()
        if self.cnt[e] >= self.EPOCH:
            self.cur[e] = self._sem("%s%d" % (e, len(self.S)))
            self.cnt[e] = 0
            if e == "pe":
                self.pe_sids.add(self.cur[e])
        self.cnt[e] += 1
        inst.then_inc(self.S[self.cur[e]], 1)
        ev = (self.cur[e], self.cnt[e])
        self.last[e] = ev
        self._mark(ev, r, w)
        self.ninst += 1
        return inst

    def dma(self, q, out, in_, r=(), w=(), **kw):
        slot = self.dq[q][self.dqi[q]]
        self.dqi[q] = (self.dqi[q] + 1) % len(self.dq[q])
        sid, tot = slot
        if tot:
            self.wait(q, (sid, tot))
        for dep in self._deps(r, w).items():
            self.wait(q, dep)
        inst = self.engs[q].dma_start(out=out, in_=in_, **kw)
        inst.then_inc(self.S[sid], 16)
        slot[1] = tot + 16
        self._mark((sid, tot + 16), r, w)
        self.ninst += 1
        return inst

    def barrier(self):
        evs = [ev for ev in self.last.values() if ev is not None]
        for q in self.dq:
            for sid, tot in self.dq[q]:
                if tot:
                    evs.append((sid, tot))
        for e in self.engs:
            for ev in evs:
                self.wait(e, ev)

    def sb(self, es, name, shape, dtype):
        return T(es.enter_context(self.nc.sbuf_tensor("s_" + name, list(shape), dtype)), name)

    def dram(self, name, shape, dtype, kind="Internal"):
        return T(self.nc.dram_tensor("d_" + name, list(shape), dtype, kind=kind), name)

    def init_psum(self, es):
        self._ps = [T(es.enter_context(self.nc.psum_tensor("psb%d" % i, [128, 512], F32)), "psb%d" % i) for i in range(8)]

    def psum(self, grp=None):
        if grp is None:
            p = self._ps[self._psi]
            self._psi = (self._psi + 1) % 8
            return p
        k = self._psg.get(grp, 0)
        self._psg[grp] = (k + 1) % 4
        return self._ps[k + (0 if grp == "acc" else 4)]


def _bf(ap_tensor):
    return ap_tensor.bitcast(BF16)


def build_program(debug=None):
    debug = debug or set()
    nc = bass.Bass("TRN2", target_bir_lowering=False)
    es = ExitStack()
    P = Prog(nc, es)
    P.init_psum(es)

    def din(name, shape, dt=F32):
        return T(nc.dram_tensor(name, list(shape), dt, kind="ExternalInput"), name)

    def dout(name, shape, dt=F32):
        return T(nc.dram_tensor(name, list(shape), dt, kind="ExternalOutput"), name)

    xroll = din("xroll", [L, D])
    cT = din("cT", [128, 8, 2])
    w_ada = din("w_ada", [D, 6 * D])
    b_adaT = din("b_adaT", [128, 48])
    b_ada_g = din("b_ada_g", [2, D])
    n1T = din("n1T", [128, 8])
    n2T = din("n2T", [128, 8])
    w_in = din("w_in", [D, NPROJ])
    b_inT = din("b_inT", [128, 40])
    b_in_qkv = din("b_in_qkv", [1, 1536])
    ident_in = din("ident", [128, 128])
    ctxb = din("ctxb", [256, D])
    t_FAu = din("t_FAu", [128, 2, 2, 2, 128], BF16)
    t_FAq = din("t_FAq", [64, 2, 2, 2, 128], BF16)
    t_FAf = din("t_FAf", [128, 2, 2, 2, 128], BF16)
    t_DFu = din("t_DFu", [128, 3, 128], BF16)
    t_DFq = din("t_DFq", [128, 3, 64], BF16)
    t_twb = din("t_twb", [128, 2, 2, 128], BF16)
    t_sel = din("t_sel", [128, 128])
    t_omf = din("t_omf", [128, 2, 4])
    t_featsT = din("t_featsT", [33, L])
    t_dec0 = din("t_dec0", [512, 512])
    t_decs = din("t_decs", [512, 16])
    hy_convw = din("hy_convw", [128, 12, 3])
    hy_convb = din("hy_convb", [128, 12])
    hy_skipc = din("hy_skipc", [128, 2, 4])
    hy_f1_w = din("hy_f1_w", [33, 64])
    hy_f2_w = din("hy_f2_w", [64, 64])
    hy_f3_w = din("hy_f3_w", [64, 64])
    hy_f4_w = din("hy_f4_w", [64, 2048])
    hy_fbc = din("hy_fbc", [64, 4])
    w_hy_out = din("w_hy_out", [512, D])
    w_na_out = din("w_na_out", [512, D])
    w_out = din("w_out", [D, D])
    peer_w_q = din("peer_w_q", [D, 2048])
    peer_keysT = din("peer_keysT", [128, 16, 128])
    peer_uTp = din("peer_uTp", [D, 128, 128])
    peer_v = din("peer_v", [16384, D])
    t_io128 = din("t_io128", [128, 128])
    biasx = din("biasx", [8, 128, 8, 512])
    negmask = din("negmask", [4, 128, 8, 512])
    q_norm_g = din("q_norm_g", [1, 64])
    k_norm_g = din("k_norm_g", [1, 64])
    rope = din("rope", [20 * 128, 64])
    out = dout("out", [2048, D])

    dbg = {}

    def dbg_out(name, shape, dt=F32):
        dbg[name] = dout("dbg_" + name, shape, dt)
        return dbg[name]

    ubd = nc.dram_tensor("d_ubd", [32, 128, 4, 8, 128], BF16, kind="Internal")
    vbd = nc.dram_tensor("d_vbd", [32, 128, 4, D], BF16, kind="Internal")
    ubd_res = Res("ubd")
    vbd_res = Res("vbd")
    zhyT = [P.dram("zhyT%d" % m, [128, L], BF16) for m in range(12)]

    ident = P.sb(es, "ident", [128, 128], BF16)
    identf = P.sb(es, "identf", [128, 128], F32)
    modT = P.sb(es, "modT", [128, 48, 2], F32)
    mods = P.sb(es, "mods", [128, 8, 8], F32)
    MI_G1, MI_S1, MI_GC, MI_SC, MI_G2, MI_S2 = 0, 1, 2, 3, 4, 5
    grow = P.sb(es, "grow", [128, 2, D], F32)

    P.dma("sp", identf[:], ident_in[:, :], w=[identf.res])
    P.op("dve", lambda: nc.vector.tensor_copy(out=ident[:], in_=identf[:]), r=[identf.res], w=[ident.res])

    with ExitStack() as pa:
        cTs = P.sb(pa, "cTs", [128, 8, 2], F32)
        scT = P.sb(pa, "scT", [128, 8, 2], F32)
        screp = P.sb(pa, "screp", [128, 8, 128], F32)
        badaT = P.sb(pa, "badaT", [128, 48], F32)
        bgrow = P.sb(pa, "bgrow", [128, 2, D], F32)
        n1s = P.sb(pa, "n1s", [128, 8], F32)
        n2s = P.sb(pa, "n2s", [128, 8], F32)
        wa = [P.sb(pa, "wa%d" % i, [128, 8, D], F32) for i in range(2)]
        P.dma("sp", cTs[:], cT[:, :, :], w=[cTs.res])
        P.dma("sp", badaT[:], b_adaT[:, :], w=[badaT.res])
        P.dma("sp", n1s[:], n1T[:, :], w=[n1s.res])
        P.dma("sp", n2s[:], n2T[:, :], w=[n2s.res])
        for gi in range(2):
            P.dma("sp", bgrow[:, gi, :], b_ada_g[gi:gi + 1, :].partition_broadcast(128), w=[bgrow.res])
        P.op("act", lambda: nc.scalar.activation(out=scT[:], in_=cTs[:], func=AF.Silu), r=[cTs.res], w=[scT.res])
        P.op("dve", lambda: nc.vector.tensor_copy(out=screp[:], in_=scT[:, :, 0:1].to_broadcast([128, 8, 128])),
             r=[scT.res], w=[screp.res])
        psm = P.psum()
        for i in range(6):
            wt = wa[i % 2]
            P.dma("sp", wt[:], w_ada[:, i * D:(i + 1) * D].rearrange("(kc p) n -> p kc n", p=128), w=[wt.res])
            for dc in range(8):
                j = i * 8 + dc
                for kc in range(8):
                    P.op("pe", lambda: nc.tensor.matmul(psm[:, 2 * j:2 * j + 2], lhsT=wt[:, kc, dc * 128:(dc + 1) * 128],
                                                        rhs=scT[:, kc, :], start=(kc == 0), stop=(kc == 7)),
                         r=[wt.res, scT.res], w=[psm.res])
            if i in (2, 5):
                gi = 0 if i == 2 else 1
                for half in range(2):
                    pr = P.psum()
                    for kc in range(8):
                        P.op("pe", lambda: nc.tensor.matmul(pr[:, :], lhsT=screp[:, kc, :],
                                                            rhs=wt[:, kc, half * 512:(half + 1) * 512],
                                                            start=(kc == 0), stop=(kc == 7)),
                             r=[wt.res, screp.res], w=[pr.res])
                    P.op("dve", lambda: nc.vector.tensor_tensor(out=grow[:, gi, half * 512:(half + 1) * 512], in0=pr[:, :],
                                                                in1=bgrow[:, gi, half * 512:(half + 1) * 512], op=ALU.add),
                         r=[pr.res, bgrow.res], w=[grow.res])
        for col in range(2):
            P.op("dve", lambda: nc.vector.tensor_tensor(out=modT[:, :, col], in0=psm[:, 0:96].rearrange("p (j c) -> p j c", c=2)[:, :, col],
                                                        in1=badaT[:], op=ALU.add),
                 r=[psm.res, badaT.res], w=[modT.res])

        def geff(dst, ng, sc_idx, col):
            P.op("dve", lambda: nc.vector.tensor_scalar(out=mods[:, dst, :], in0=modT[:, sc_idx * 8:(sc_idx + 1) * 8, col],
                                                        scalar1=1.0, scalar2=None, op0=ALU.add),
                 r=[modT.res], w=[mods.res])
            P.op("dve", lambda: nc.vector.tensor_tensor(out=mods[:, dst, :], in0=mods[:, dst, :], in1=ng[:], op=ALU.mult),
                 r=[mods.res, ng.res], w=[mods.res])

        def cpy(dst, idx, col):
            P.op("dve", lambda: nc.vector.tensor_copy(out=mods[:, dst, :], in_=modT[:, idx * 8:(idx + 1) * 8, col]),
                 r=[modT.res], w=[mods.res])

        geff(MI_G1, n1s, 1, 0)
        cpy(MI_S1, 0, 0)
        geff(MI_GC, n1s, 1, 1)
        cpy(MI_SC, 0, 1)
        geff(MI_G2, n2s, 4, 0)
        cpy(MI_S2, 3, 0)
        P.barrier()

    if "modT" in debug:
        d1 = dbg_out("modT", [128, 96])
        d2 = dbg_out("grow", [128, 2 * D])
        P.dma("sp", d1[:, :], modT[:].rearrange("p j c -> p (j c)"), r=[modT.res], w=[d1.res])
        P.dma("sp", d2[:, :], grow[:].rearrange("p g n -> p (g n)"), r=[grow.res], w=[d2.res])

    if "stopA" in debug:
        P.barrier()
        return nc, es, dbg

    NA_T = 22
    attTd = P.dram("attTd", [128, 4, 2048], BF16)
    pbc = ExitStack()
    qT = P.sb(pbc, "qT", [128, 4, 2048], BF16)
    kT = P.sb(pbc, "kT", [128, 4, NA_T * 128], BF16)
    vz = P.sb(pbc, "vz", [128, NA_T, 512], BF16)
    gsT = [P.dram("gsT%d" % m, [128, 2048], BF16) for m in range(16)]

    with ExitStack() as pb:
        winb = P.sb(pb, "winb", [128, 8, NPROJ], BF16)
        binT = P.sb(pb, "binT", [128, 40], F32)
        bqkv = P.sb(pb, "bqkv", [128, 1536], F32)
        qg = P.sb(pb, "qg", [128, 64], F32)
        kg = P.sb(pb, "kg", [128, 64], F32)
        P.dma("sp", binT[:], b_inT[:, :], w=[binT.res])
        P.dma("sp", bqkv[:], b_in_qkv[0:1, :].partition_broadcast(128), w=[bqkv.res])
        P.dma("sp", qg[:], q_norm_g[0:1, :].partition_broadcast(128), w=[qg.res])
        P.dma("sp", kg[:], k_norm_g[0:1, :].partition_broadcast(128), w=[kg.res])
        xt = [P.sb(pb, "xt%d" % i, [128, D], F32) for i in range(2)]
        for pc in range(40):
            ws = xt[pc % 2]
            P.dma("sp", ws[:].rearrange("p (kc n) -> p kc n", kc=8), w_in[:, pc * 128:(pc + 1) * 128].rearrange("(kc p) n -> p kc n", p=128), w=[ws.res])
            eng = "dve" if pc % 2 == 0 else "pool"
            P.op(eng, lambda: P.engs[eng].tensor_copy(out=winb[:, :, pc * 128:(pc + 1) * 128], in_=ws[:].rearrange("p (kc n) -> p kc n", kc=8)),
                 r=[ws.res], w=[winb.res])

        junk = P.sb(pb, "junk", [128, D], BF16)
        xn = [P.sb(pb, "xn%d" % i, [128, D], BF16) for i in range(2)]
        st4 = [P.sb(pb, "st4_%d" % i, [128, 4], F32) for i in range(2)]
        hTb = [P.sb(pb, "hT%d" % i, [128, 8, 512], BF16) for i in range(2)]
        stg = [P.sb(pb, "stg%d" % i, [128, 512], BF16) for i in range(4)]
        qkv = [P.sb(pb, "qkv%d" % i, [128, 1536], F32) for i in range(1)]
        sq = P.sb(pb, "sq", [128, 512], F32)
        st8 = P.sb(pb, "st8", [128, 4, 8], F32)
        qn = P.sb(pb, "qn", [128, 512], F32)
        rt = [P.sb(pb, "rt%d" % i, [128, 4, 256], F32) for i in range(1)]
        qr = [P.sb(pb, "qr%d" % i, [128, 512], BF16) for i in range(2)]
        ropet = [P.sb(pb, "ropet%d" % i, [128, 64], F32) for i in range(2)]
        cnt = {"stg": 0, "ev": 0, "tile": 0, "qk": 0}

        def norm_tile(src_ap, gi, si, hT, tt):
            k = cnt["tile"]
            cnt["tile"] += 1
            x_ = xt[k % 2]
            xn_ = xn[k % 2]
            s_ = st4[k % 2]
            P.dma("sp", x_[:], src_ap, w=[x_.res])
            P.op("act", lambda: nc.scalar.activation(out=junk[:], in_=x_[:], func=AF.Square, accum_out=s_[:, 0:1]),
                 r=[x_.res], w=[junk.res, s_.res])
            P.op("dve", lambda: nc.vector.tensor_scalar(out=s_[:, 1:2], in0=s_[:, 0:1], scalar1=1.0 / D, scalar2=EPS,
                                                        op0=ALU.mult, op1=ALU.add), r=[s_.res], w=[s_.res])
            P.op("act", lambda: nc.scalar.activation(out=s_[:, 2:3], in_=s_[:, 1:2], func=AF.Sqrt), r=[s_.res], w=[s_.res])
            P.op("dve", lambda: nc.vector.reciprocal(out=s_[:, 3:4], in_=s_[:, 2:3]), r=[s_.res], w=[s_.res])
            P.op("dve", lambda: nc.vector.tensor_scalar(out=xn_[:], in0=x_[:], scalar1=s_[:, 3:4], scalar2=None, op0=ALU.mult),
                 r=[x_.res, s_.res], w=[xn_.res])
            pb_ = P.psum()
            pbf = pb_.t.bitcast(BF16)
            for kc in range(8):
                P.op("pe", lambda: nc.tensor.transpose(out=pbf[:, kc * 128:(kc + 1) * 128], in_=xn_[:, kc * 128:(kc + 1) * 128],
                                                       identity=ident[:]), r=[xn_.res, ident.res], w=[pb_.res])
            for kc in range(8):
                P.op("act", lambda: nc.scalar.activation(out=hT[:, kc, tt * 128:(tt + 1) * 128], in_=pbf[:, kc * 128:(kc + 1) * 128],
                                                         func=AF.Identity, scale=mods[:, gi, kc:kc + 1], bias=mods[:, si, kc:kc + 1]),
                     r=[pb_.res, mods.res], w=[hT.res])

        def fm_chunk(hT, ncols, m, func, dst_ap, dst_res):
            ps = P.psum()
            for kc in range(8):
                P.op("pe", lambda: nc.tensor.matmul(ps[:, :ncols], lhsT=winb[:, kc, m * 128:(m + 1) * 128], rhs=hT[:, kc, :ncols],
                                                    start=(kc == 0), stop=(kc == 7)), r=[winb.res, hT.res], w=[ps.res])
            sg = stg[cnt["stg"] % 4]
            cnt["stg"] += 1
            if func is None and cnt["ev"] % 2 == 0:
                P.op("dve", lambda: nc.vector.tensor_scalar(out=sg[:, :ncols], in0=ps[:, :ncols], scalar1=binT[:, m:m + 1], scalar2=None,
                                                            op0=ALU.add), r=[ps.res, binT.res], w=[sg.res])
            else:
                P.op("act", lambda: nc.scalar.activation(out=sg[:, :ncols], in_=ps[:, :ncols], func=(func or AF.Identity),
                                                         bias=binT[:, m:m + 1]), r=[ps.res, binT.res], w=[sg.res])
            cnt["ev"] += 1
            P.dma("pool", dst_ap, sg[:, :ncols], r=[sg.res], w=[dst_res])

        def headnorm_rope(src, qkvc, gain, rope_ap, dst):
            P.op("act", lambda: nc.scalar.activation(out=sq[:], in_=src, func=AF.Square), r=[qkvc.res], w=[sq.res])
            P.op("dve", lambda: nc.vector.tensor_reduce(out=st8[:, 0, :], in_=sq[:].rearrange("p (h d) -> p h d", d=64),
                                                        axis=AX.X, op=ALU.add), r=[sq.res], w=[st8.res])
            P.op("dve", lambda: nc.vector.tensor_scalar(out=st8[:, 1, :], in0=st8[:, 0, :], scalar1=1.0 / 64, scalar2=EPS,
                                                        op0=ALU.mult, op1=ALU.add), r=[st8.res], w=[st8.res])
            P.op("act", lambda: nc.scalar.activation(out=st8[:, 2, :], in_=st8[:, 1, :], func=AF.Sqrt), r=[st8.res], w=[st8.res])
            P.op("dve", lambda: nc.vector.reciprocal(out=st8[:, 3, :], in_=st8[:, 2, :]), r=[st8.res], w=[st8.res])
            qn3 = qn[:].rearrange("p (h d) -> p h d", d=64)
            P.op("dve", lambda: nc.vector.tensor_tensor(out=qn3, in0=src.rearrange("p (h d) -> p h d", d=64),
                                                        in1=st8[:, 3, :].unsqueeze(2).to_broadcast([128, 8, 64]), op=ALU.mult),
                 r=[qkvc.res, st8.res], w=[qn.res])
            if rope_ap is None:
                P.op("dve", lambda: nc.vector.tensor_tensor(out=dst[:].rearrange("p (h d) -> p h d", d=64), in0=qn3,
                                                            in1=gain[:].unsqueeze(1).to_broadcast([128, 8, 64]), op=ALU.mult),
                     r=[qn.res, gain.res], w=[dst.res])
                return
            P.op("dve", lambda: nc.vector.tensor_tensor(out=qn3, in0=qn3, in1=gain[:].unsqueeze(1).to_broadcast([128, 8, 64]),
                                                        op=ALU.mult), r=[qn.res, gain.res], w=[qn.res])
            q5 = qn[:].rearrange("p (h a b f) -> p h a b f", h=8, a=2, b=2)
            d5 = dst[:].rearrange("p (h a b f) -> p h a b f", h=8, a=2, b=2)
            A, B_ = q5[:, :, :, 0, :], q5[:, :, :, 1, :]
            C = rope_ap[:, 0:32].rearrange("p (a f) -> p a f", a=2).unsqueeze(1).to_broadcast([128, 8, 2, 16])
            S_ = rope_ap[:, 32:64].rearrange("p (a f) -> p a f", a=2).unsqueeze(1).to_broadcast([128, 8, 2, 16])
            r_ = rt[0]
            tv = [r_[:, i, :].rearrange("p (h a f) -> p h a f", h=8, a=2) for i in range(4)]
            for o_, i0, i1 in ((tv[0], A, C), (tv[1], B_, S_), (tv[2], A, S_), (tv[3], B_, C)):
                P.op("dve", lambda: nc.vector.tensor_tensor(out=o_, in0=i0, in1=i1, op=ALU.mult),
                     r=[qn.res, rope_ap.res], w=[r_.res])
            P.op("dve", lambda: nc.vector.tensor_tensor(out=d5[:, :, :, 0, :], in0=tv[0], in1=tv[1], op=ALU.subtract),
                 r=[r_.res], w=[dst.res])
            P.op("dve", lambda: nc.vector.tensor_tensor(out=d5[:, :, :, 1, :], in0=tv[2], in1=tv[3], op=ALU.add),
                 r=[r_.res], w=[dst.res])

        def to_T(src_bf, dstT, col0):
            ps = P.psum()
            pbf = ps.t.bitcast(BF16)
            for a in range(4):
                P.op("pe", lambda: nc.tensor.transpose(out=pbf[:, a * 128:(a + 1) * 128], in_=src_bf[:, a * 128:(a + 1) * 128],
                                                       identity=ident[:]), r=[src_bf.res, ident.res], w=[ps.res])
            P.op("act", lambda: nc.scalar.copy(out=dstT[:, :, col0:col0 + 128], in_=pbf[:, 0:512].rearrange("p (a t) -> p a t", a=4)),
                 r=[ps.res], w=[dstT.res])

        def tm_qkv(hT, tt, nb, parts, is_ctx):
            qkvc = qkv[0]
            ropec = ropet[cnt["qk"] % 2]
            cnt["qk"] += 1
            for part in parts:
                ps = P.psum()
                for kc in range(8):
                    P.op("pe", lambda: nc.tensor.matmul(ps[:, :], lhsT=hT[:, kc, tt * 128:(tt + 1) * 128],
                                                        rhs=winb[:, kc, 1536 + part * 512:1536 + (part + 1) * 512],
                                                        start=(kc == 0), stop=(kc == 7)), r=[winb.res, hT.res], w=[ps.res])
                P.op("dve", lambda: nc.vector.tensor_tensor(out=qkvc[:, part * 512:(part + 1) * 512], in0=ps[:, :],
                                                            in1=bqkv[:, part * 512:(part + 1) * 512], op=ALU.add),
                     r=[ps.res, bqkv.res], w=[qkvc.res])
            if not is_ctx:
                P.dma("sp", ropec[:], rope[nb * 128:(nb + 1) * 128, :], w=[ropec.res])
            if 0 in parts:
                qd = qr[0]
                headnorm_rope(qkvc[:, 0:512], qkvc, qg, ropec, qd)
                to_T(qd, qT, (nb - 2) * 128)
            kd = qr[1]
            headnorm_rope(qkvc[:, 512:1024], qkvc, kg, (None if is_ctx else ropec), kd)
            to_T(kd, kT, nb * 128)
            P.op("pool", lambda: nc.gpsimd.tensor_copy(out=vz[:, nb, :], in_=qkvc[:, 1024:1536]), r=[qkvc.res], w=[vz.res])

        for blk in range(16):
            hT = hTb[blk % 2]
            for tt in range(4):
                i = blk * 4 + tt
                norm_tile(xroll[i * 128:(i + 1) * 128, :], MI_G1, MI_S1, hT, tt)
            for m in range(12):
                fm_chunk(hT, 512, m, None, zhyT[m][:, blk * 512:(blk + 1) * 512], zhyT[m].res)
            if blk < 4:
                for m in range(24, 40):
                    fm_chunk(hT, 512, m, AF.Sigmoid, gsT[m - 24][:, blk * 512:(blk + 1) * 512], gsT[m - 24].res)
            for tt in range(4):
                i = blk * 4 + tt
                if i <= 17:
                    tm_qkv(hT, tt, i + 2, (0, 1, 2) if i < 16 else (1, 2), False)
                elif i >= 62:
                    tm_qkv(hT, tt, i - 62, (1, 2), False)
        hT = hTb[0]
        for tt in range(2):
            norm_tile(ctxb[tt * 128:(tt + 1) * 128, :], MI_GC, MI_SC, hT, tt)
        for tt in range(2):
            tm_qkv(hT, tt, 20 + tt, (1, 2), True)
        P.barrier()

    if "phaseB" in debug:
        d1 = dbg_out("zhyT", [12 * 128, L], BF16)
        d2 = dbg_out("gsT", [16 * 128, 2048], BF16)
        d3 = dbg_out("qT", [128, 4 * 2048], BF16)
        d4 = dbg_out("kT", [128, 4 * NA_T * 128], BF16)
        d5 = dbg_out("vz", [128, NA_T * 512], BF16)
        for m in range(12):
            P.dma("sp", d1[m * 128:(m + 1) * 128, :], zhyT[m][:, :], r=[zhyT[m].res], w=[d1.res])
        for m in range(16):
            P.dma("sp", d2[m * 128:(m + 1) * 128, :], gsT[m][:, :], r=[gsT[m].res], w=[d2.res])
        P.dma("sp", d3[:, :], qT[:].rearrange("p a t -> p (a t)"), r=[qT.res], w=[d3.res])
        P.dma("sp", d4[:, :], kT[:].rearrange("p a t -> p (a t)"), r=[kT.res], w=[d4.res])
        P.dma("sp", d5[:, :], vz[:].rearrange("p t d -> p (t d)"), r=[vz.res], w=[d5.res])
    if "stopB" in debug:
        P.barrier()
        return nc, es, dbg

    with ExitStack() as pc_:
        attT = P.sb(pc_, "attT", [128, 4, 2048], BF16)
        onesb = P.sb(pc_, "onesb", [128, 128], BF16)
        P.op("dve", lambda: nc.vector.memset(onesb[:], 1.0), w=[onesb.res])
        bx = [P.sb(pc_, "bx%d" % i, [128, 8, 512], F32) for i in range(2)]
        nm = P.sb(pc_, "nm", [128, 8, 512], F32)
        bm = [P.sb(pc_, "bm%d" % i, [128, 8, 512], F32) for i in range(2)]
        tt_ = [P.sb(pc_, "ttc%d" % i, [128, 512], F32) for i in range(2)]
        pT = [P.sb(pc_, "pT%d" % i, [128, 512], BF16) for i in range(3)]
        rden = P.sb(pc_, "rden", [128, 512], F32)
        cvf = [P.sb(pc_, "cvf%d" % i, [128, 2048], F32) for i in range(2)]
        cvb = [P.sb(pc_, "cvb%d" % i, [128, 2048], BF16) for i in range(2)]
        cvn = {"k": 0}

        def conv_steps(n):
            for _ in range(n):
                k = cvn["k"]
                if k >= 128:
                    return
                cvn["k"] += 1
                f_, b_ = cvf[k % 2], cvb[k % 2]
                if k < 64:
                    for j2 in range(2):
                        P.dma("sp", f_[:, j2 * 1024:(j2 + 1) * 1024].rearrange("p (kc i) -> p kc i", kc=8),
                              peer_uTp[:, 2 * k + j2, :].rearrange("(kc p) i -> p kc i", p=128), w=[f_.res])
                else:
                    P.dma("sp", f_[:].rearrange("p (j d) -> p j d", j=2),
                          peer_v.t.ap().rearrange("(i1 i2) d -> i1 i2 d", i2=128)[:, 2 * (k - 64):2 * (k - 64) + 2, :], w=[f_.res])
                P.op("pool", lambda: nc.gpsimd.tensor_copy(out=b_[:], in_=f_[:]), r=[f_.res], w=[b_.res])
                if k < 64:
                    P.dma("pool", ubd[k // 2, :, 2 * (k % 2):2 * (k % 2) + 2, :, :].rearrange("p j kc i -> p (j kc i)"), b_[:], r=[b_.res], w=[ubd_res])
                else:
                    kk = k - 64
                    P.dma("pool", vbd[kk // 2, :, 2 * (kk % 2):2 * (kk % 2) + 2, :].rearrange("p j d -> p (j d)"), b_[:], r=[b_.res], w=[vbd_res])

        it = 0
        for j in range(4):
            P.dma("sp", nm[:], negmask[j, :, :, :], w=[nm.res])
            for h in range(8):
                a, hb = h // 2, (h % 2) * 64
                bx_ = bx[it % 2]
                bm_ = bm[it % 2]
                it += 1
                P.dma("sp", bx_[:], biasx[h, :, :, :], w=[bx_.res])
                P.op("dve", lambda: nc.vector.tensor_tensor(out=bm_[:], in0=bx_[:], in1=nm[:], op=ALU.add),
                     r=[bx_.res, nm.res], w=[bm_.res])
                conv_steps(4)
                psN = P.psum("acc")
                psD = P.psum("acc")
                def s_c(c):
                    tile_i = (4 * j + c) if c < 8 else (20 + c - 8)
                    psS = P.psum("tmp")
                    P.op("pe", lambda: nc.tensor.matmul(psS[:, :], lhsT=kT[hb:hb + 64, a, tile_i * 128:(tile_i + 1) * 128],
                                                        rhs=qT[hb:hb + 64, a, j * 512:(j + 1) * 512], start=True, stop=True),
                         r=[kT.res, qT.res], w=[psS.res])
                    p_ = pT[c % 3]
                    if c < 8:
                        t_ = tt_[c % 2]
                        P.op("dve", lambda: nc.vector.scalar_tensor_tensor(out=t_[:], in0=psS[:, :], scalar=0.125, in1=bm_[:, c, :],
                                                                           op0=ALU.mult, op1=ALU.add),
                             r=[psS.res, bm_.res], w=[t_.res])
                        P.op("act", lambda: nc.scalar.activation(out=p_[:], in_=t_[:], func=AF.Exp), r=[t_.res], w=[p_.res])
                    else:
                        P.op("act", lambda: nc.scalar.activation(out=p_[:], in_=psS[:, :], func=AF.Exp, scale=0.125),
                             r=[psS.res], w=[p_.res])
                    return p_, tile_i

                def pv_c(c, st_):
                    p_, tile_i = st_
                    P.op("pe", lambda: nc.tensor.matmul(psN[hb:hb + 64, :], lhsT=vz[:, tile_i, h * 64:(h + 1) * 64], rhs=p_[:],
                                                        start=(c == 0), stop=(c == 9)), r=[vz.res, p_.res], w=[psN.res])
                    P.op("pe", lambda: nc.tensor.matmul(psD[:, :], lhsT=onesb[:], rhs=p_[:], start=(c == 0), stop=(c == 9)),
                         r=[onesb.res, p_.res], w=[psD.res])

                prev = None
                for c in range(10):
                    cur = s_c(c)
                    if prev is not None:
                        pv_c(c - 1, prev)
                    prev = cur
                pv_c(9, prev)
                P.op("dve", lambda: nc.vector.reciprocal(out=rden[hb:hb + 64, :], in_=psD[hb:hb + 64, :]), r=[psD.res], w=[rden.res])
                P.op("dve", lambda: nc.vector.tensor_tensor(out=attT[hb:hb + 64, a, j * 512:(j + 1) * 512], in0=psN[hb:hb + 64, :],
                                                            in1=rden[hb:hb + 64, :], op=ALU.mult),
                     r=[psN.res, rden.res], w=[attT.res])
        P.dma("sp", attTd.t.ap(), attT[:], r=[attT.res], w=[attTd.res])
        P.barrier()

    pbc.close()
    if "phaseC" in debug:
        d1 = dbg_out("attT", [128, 4 * 2048], BF16)
        P.dma("sp", d1[:, :], attTd.t.ap().rearrange("p a t -> p (a t)"), r=[attTd.res], w=[d1.res])
    if "stopC" in debug:
        P.barrier()
        return nc, es, dbg

    CS = 64
    NFFT = 16384
    hyTd = [P.dram("hyTd%d" % g, [128, 2048], BF16) for g in range(4)]
    with ExitStack() as pd:
        FAu = P.sb(pd, "FAu", [128, 2, 2, 2, 128], BF16)
        FAq = P.sb(pd, "FAq", [64, 2, 2, 2, 128], BF16)
        FAf = P.sb(pd, "FAf", [128, 2, 2, 2, 128], BF16)
        DFu = P.sb(pd, "DFu", [128, 3, 128], BF16)
        DFq = P.sb(pd, "DFq", [128, 3, 64], BF16)
        twb = P.sb(pd, "twb", [128, 2, 2, 128], BF16)
        sel = P.sb(pd, "sel", [128, 128], F32)
        convw = P.sb(pd, "convw", [128, 12, 3], F32)
        convb = P.sb(pd, "convb", [128, 12], F32)
        omf = P.sb(pd, "omf", [128, 2, 4], F32)
        wedge = P.sb(pd, "wedge", [128, 2, 12, 4], F32)
        skipc = P.sb(pd, "skipc", [128, 2, 4], F32)
        h3 = P.sb(pd, "h3", [64, L], BF16)
        W4b = P.sb(pd, "W4b", [64, 2048], BF16)
        for dst, src in ((FAu, t_FAu), (FAq, t_FAq), (FAf, t_FAf), (DFu, t_DFu), (DFq, t_DFq), (twb, t_twb), (sel, t_sel),
                         (convw, hy_convw), (convb, hy_convb), (omf, t_omf), (skipc, hy_skipc)):
            P.dma("sp", dst.t.ap(), src.t.ap(), w=[dst.res])
        for e_ in range(2):
            for m in range(12):
                P.op("dve", lambda: nc.vector.tensor_scalar(out=wedge[:, e_, m, :], in0=omf[:, e_, :], scalar1=convw[:, m, 2 * e_:2 * e_ + 1],
                                                            scalar2=None, op0=ALU.mult), r=[omf.res, convw.res], w=[wedge.res])

        with ExitStack() as pm:
            featsT = P.sb(pm, "featsT", [33, L], F32)
            hA = P.sb(pm, "hA", [64, L], F32)
            hB = P.sb(pm, "hB", [64, L], F32)
            W1 = P.sb(pm, "W1", [33, 64], F32)
            W2 = P.sb(pm, "W2", [64, 64], F32)
            W3 = P.sb(pm, "W3", [64, 64], F32)
            fbc = P.sb(pm, "fbc", [64, 4], F32)
            fb = P.sb(pm, "fb", [64, 3], F32)
            W4f = P.sb(pm, "W4f", [64, 2048], F32)
            ut = [P.sb(pm, "ut%d" % i, [64, 512], F32) for i in range(2)]
            kt = [P.sb(pm, "kt%d" % i, [64, 512], F32) for i in range(2)]
            P.dma("sp", featsT[:], t_featsT[:, :], w=[featsT.res])
            P.dma("sp", W1[:], hy_f1_w[:, :], w=[W1.res])
            P.dma("sp", W2[:], hy_f2_w[:, :], w=[W2.res])
            P.dma("sp", W3[:], hy_f3_w[:, :], w=[W3.res])
            P.dma("sp", fbc[:], hy_fbc[:, :], w=[fbc.res])
            P.dma("sp", W4f[:], hy_f4_w[:, :], w=[W4f.res])
            for o_ in range(2):
                P.op("dve", lambda: nc.vector.tensor_copy(
                    out=W4b[:, o_ * 1024:(o_ + 1) * 1024].rearrange("p (cc d c) -> p cc d c", cc=8, d=2),
                    in_=W4f[:, o_ * 1024:(o_ + 1) * 1024].rearrange("p (d cc c) -> p cc d c", d=2, cc=8)), r=[W4f.res], w=[W4b.res])
            P.op("dve", lambda: nc.vector.tensor_scalar(out=fb[:], in0=fbc[:, 0:3], scalar1=fbc[:, 3:4], scalar2=None, op0=ALU.mult),
                 r=[fbc.res], w=[fb.res])
            MAGIC = 12582912.0
            TWO_PI = 2.0 * math.pi

            def mlp_layer(src, K, Wt, li, dst):
                for blk in range(16):
                    cols = slice(blk * 512, (blk + 1) * 512)
                    ps = P.psum("tmp")
                    P.op("pe", lambda: nc.tensor.matmul(ps[:64, :], lhsT=Wt[:K, :], rhs=src[:K, cols], start=True, stop=True),
                         r=[Wt.res, src.res], w=[ps.res])
                    u = ut[blk % 2]
                    k_ = kt[blk % 2]
                    P.op("dve", lambda: nc.vector.tensor_scalar(out=u[:], in0=ps[:64, :], scalar1=fbc[:, 3:4], scalar2=fb[:, li:li + 1],
                                                                op0=ALU.mult, op1=ALU.add), r=[ps.res, fbc.res, fb.res], w=[u.res])
                    P.op("dve", lambda: nc.vector.tensor_scalar(out=k_[:], in0=u[:], scalar1=1.0 / TWO_PI, scalar2=MAGIC,
                                                                op0=ALU.mult, op1=ALU.add), r=[u.res], w=[k_.res])
                    P.op("dve", lambda: nc.vector.tensor_scalar(out=k_[:], in0=k_[:], scalar1=-MAGIC, scalar2=None, op0=ALU.add),
                         r=[k_.res], w=[k_.res])
                    P.op("dve", lambda: nc.vector.scalar_tensor_tensor(out=u[:], in0=k_[:], scalar=-TWO_PI, in1=u[:], op0=ALU.mult, op1=ALU.add),
                         r=[k_.res, u.res], w=[u.res])
                    P.op("dve", lambda: nc.vector.tensor_scalar(out=u[:], in0=u[:], scalar1=3.141592, scalar2=-3.141592,
                                                                op0=ALU.min, op1=ALU.max), r=[u.res], w=[u.res])
                    P.op("act", lambda: nc.scalar.activation(out=dst[:, cols], in_=u[:], func=AF.Sin), r=[u.res], w=[dst.res])

            mlp_layer(featsT, 33, W1, 0, hA)
            mlp_layer(hA, 64, W2, 1, hB)
            mlp_layer(hB, 64, W3, 2, h3)
            P.barrier()

        if "h3" in debug:
            d1 = dbg_out("h3", [64, L], BF16)
            P.dma("sp", d1[:, :], h3[:], r=[h3.res], w=[d1.res])

        vc = P.sb(pd, "vc", [128, L], BF16)
        x1c = P.sb(pd, "x1c", [128, L], BF16)
        y1 = P.sb(pd, "y1", [128, L], BF16)
        x2c = P.sb(pd, "x2c", [128, 2048], BF16)
        Xin = P.sb(pd, "Xin", [128, CS, 128], BF16)
        Abuf = P.sb(pd, "Abuf", [128, 2, CS, 64], BF16)
        Kf = P.sb(pd, "Kf", [128, 2, CS, 128], BF16)
        Ybuf = P.sb(pd, "Ybuf", [128, 2, CS, 128], BF16)
        hf = T(Ybuf[:, 0, :, :].rearrange("p c k -> p (c k)"), "hf")
        WK = [P.sb(pd, "WK%d" % i, [128, 2, 512], BF16) for i in range(4)]
        dec0 = P.sb(pd, "dec0", [128, 512], F32)
        decs = P.sb(pd, "decs", [128, 16], F32)
        acc = P.sb(pd, "acc", [128, 20], F32)
        rtot = P.sb(pd, "rtot", [128, 2], F32)
        junkh = P.sb(pd, "junkh", [128, 512], BF16)
        gt = [P.sb(pd, "gt%d" % i, [128, 1024], F32) for i in range(1)]
        ctmp = gt[0]
        cn = {"g": 0, "e": 0}
        def short_conv(src_rows, m, dst, nblk):
            P.dma("sp", hf[:, :], src_rows.t.ap(), r=[src_rows.res], w=[hf.res])
            W_ = 1024
            for blk in range(nblk):
                c0 = blk * W_
                P.op("dve", lambda: nc.vector.tensor_scalar(out=ctmp[:, :], in0=hf[:, c0:c0 + W_], scalar1=convw[:, m, 1:2], scalar2=convb[:, m:m + 1],
                                                            op0=ALU.mult, op1=ALU.add), r=[hf.res, convw.res, convb.res], w=[ctmp.res])
                P.op("dve", lambda: nc.vector.scalar_tensor_tensor(out=ctmp[:, 1:W_], in0=hf[:, c0:c0 + W_ - 1], scalar=convw[:, m, 0:1],
                                                                   in1=ctmp[:, 1:W_], op0=ALU.mult, op1=ALU.add),
                     r=[hf.res, convw.res, ctmp.res], w=[ctmp.res])
                P.op("dve", lambda: nc.vector.scalar_tensor_tensor(out=ctmp[:, 0:W_ - 1], in0=hf[:, c0 + 1:c0 + W_], scalar=convw[:, m, 2:3],
                                                                   in1=ctmp[:, 0:W_ - 1], op0=ALU.mult, op1=ALU.add),
                     r=[hf.res, convw.res, ctmp.res], w=[ctmp.res])
                lft = (c0 - 1) % L
                rgt = (c0 + W_) % L
                sl = wedge[:, 0, m, (c0 // 2048):(c0 // 2048) + 1] if c0 % 2048 == 0 else convw[:, m, 0:1]
                sr = wedge[:, 1, m, (c0 // 2048):(c0 // 2048) + 1] if (c0 + W_) % 2048 == 0 else convw[:, m, 2:3]
                P.op("dve", lambda: nc.vector.scalar_tensor_tensor(out=ctmp[:, 0:1], in0=hf[:, lft:lft + 1], scalar=sl,
                                                                   in1=ctmp[:, 0:1], op0=ALU.mult, op1=ALU.add),
                     r=[hf.res, wedge.res, convw.res, ctmp.res], w=[ctmp.res])
                P.op("dve", lambda: nc.vector.scalar_tensor_tensor(out=ctmp[:, W_ - 1:W_], in0=hf[:, rgt:rgt + 1], scalar=sr,
                                                                   in1=ctmp[:, W_ - 1:W_], op0=ALU.mult, op1=ALU.add),
                     r=[hf.res, wedge.res, convw.res, ctmp.res], w=[ctmp.res])
                P.op("act", lambda: nc.scalar.copy(out=dst[:, c0:c0 + W_], in_=ctmp[:, :]), r=[ctmp.res], w=[dst.res])

        def filter_gen(c0abs, o):
            lh = W4b[:, o * 1024 + (c0abs // 64) * 128:o * 1024 + (c0abs // 64) * 128 + 128]
            for blk in range(16):
                cols = slice(blk * 512, (blk + 1) * 512)
                ps = P.psum("tmp")
                P.op("pe", lambda: nc.tensor.matmul(ps[:, :], lhsT=lh, rhs=h3[:, cols], start=True, stop=True),
                     r=[W4b.res, h3.res], w=[ps.res])
                P.op("dve", lambda: nc.vector.scalar_tensor_tensor(out=hf[:, cols], in0=ps[:, :], scalar=decs[:, blk:blk + 1], in1=dec0[:],
                                                                   op0=ALU.mult, op1=ALU.mult), r=[ps.res, decs.res, dec0.res], w=[hf.res])
                P.op("act", lambda: nc.scalar.activation(out=junkh[:], in_=hf[:, cols], func=AF.Abs, accum_out=acc[:, blk:blk + 1]),
                     r=[hf.res], w=[junkh.res, acc.res])
            P.op("dve", lambda: nc.vector.tensor_reduce(out=acc[:, 16:17], in_=acc[:, 0:16], axis=AX.X, op=ALU.add), r=[acc.res], w=[acc.res])
            ps = P.psum("tmp")
            P.op("pe", lambda: nc.tensor.matmul(ps[:, 0:1], lhsT=sel[:], rhs=acc[:, 16:17], start=True, stop=True),
                 r=[sel.res, acc.res], w=[ps.res])
            P.op("dve", lambda: nc.vector.reciprocal(out=rtot[:, o:o + 1], in_=ps[:, 0:1]), r=[ps.res], w=[rtot.res])
            P.op("dve", lambda: nc.vector.memset(hf[64:128, 0:1], 0.0), w=[hf.res])

        def to_xin(S_, cs):
            Sv = S_[cs:cs + 64, :].rearrange("p (t l) -> p l t", l=128)
            for g16 in range(8):
                ps = P.psum("tmp")
                pbf = ps.t.bitcast(BF16)
                for s_ in range(16):
                    lo = g16 * 16 + s_
                    P.op("pe", lambda: nc.tensor.transpose(out=pbf[:64, s_ * 64:(s_ + 1) * 64], in_=Sv[:, lo, :], identity=ident[cs:cs + 64, cs:cs + 64]),
                         r=[S_.res, ident.res], w=[ps.res])
                eng = "act" if g16 % 2 == 0 else "dve"
                o_ = Xin[:64, :, g16 * 16:(g16 + 1) * 16]
                i_ = pbf[:64, 0:1024].rearrange("p (l c) -> p c l", c=64)
                if eng == "act":
                    P.op("act", lambda: nc.scalar.copy(out=o_, in_=i_), r=[ps.res], w=[Xin.res])
                else:
                    P.op("dve", lambda: nc.vector.tensor_copy(out=o_, in_=i_), r=[ps.res], w=[Xin.res])

        AbR = [Res("Ab%d" % i) for i in range(CS // 8)]
        KfR = [Res("Kf%d" % i) for i in range(CS // 8)]
        YbR = [Res("Yb%d" % i) for i in range(CS // 8)]
        hf.res.subs = YbR

        def to_xin_filter():
            Sf = hf[0:64, :].rearrange("p (t l) -> p l t", l=128)
            Sb = hf[64:128, :].rearrange("p (t l) -> p l t", l=128)
            P.op("pool", lambda: nc.gpsimd.memset(Xin[96:128, :, 0:1], 0.0), w=[Xin.res])
            for g16 in range(8):
                ps = P.psum("tmp")
                pbf = ps.t.bitcast(BF16)
                for s_ in range(16):
                    lo = g16 * 16 + s_
                    P.op("pe", lambda: nc.tensor.transpose(out=pbf[0:64, s_ * 64:(s_ + 1) * 64], in_=Sf[:, lo, :], identity=ident[0:64, 0:64]),
                         r=[hf.res, ident.res], w=[ps.res])
                    if lo == 0:
                        P.op("pe", lambda: nc.tensor.transpose(out=pbf[64:127, 0:64], in_=Sb[:, 0, 1:64], identity=ident[64:128, 64:128]),
                             r=[hf.res, ident.res], w=[ps.res])
                    else:
                        P.op("pe", lambda: nc.tensor.transpose(out=pbf[64:128, s_ * 64:(s_ + 1) * 64], in_=Sb[:, 128 - lo, :], identity=ident[64:128, 64:128]),
                             r=[hf.res, ident.res], w=[ps.res])
                o_ = Xin[:, :, g16 * 16:(g16 + 1) * 16]
                i_ = pbf[:, 0:1024].rearrange("p (l c) -> p c l", c=64)
                if g16 == 0:
                    P.op("act", lambda: nc.scalar.copy(out=Xin[0:64, :, 0:16], in_=pbf[0:64, 0:1024].rearrange("p (l c) -> p c l", c=64)), r=[ps.res], w=[Xin.res])
                    P.op("dve", lambda: nc.vector.tensor_copy(out=Xin[64:128, :, 1:16], in_=pbf[64:128, 64:1024].rearrange("p (l c) -> p c l", c=64)), r=[ps.res], w=[Xin.res])
                    P.op("dve", lambda: nc.vector.tensor_copy(out=Xin[64:127, :, 0:1], in_=pbf[64:127, 0:64].rearrange("p (l c) -> p c l", c=64)), r=[ps.res], w=[Xin.res])
                elif g16 % 2 == 0:
                    P.op("act", lambda: nc.scalar.copy(out=o_, in_=i_), r=[ps.res], w=[Xin.res])
                else:
                    P.op("dve", lambda: nc.vector.tensor_copy(out=o_, in_=i_), r=[ps.res], w=[Xin.res])

        def stage_a(inp_fn, in_res_fn, K, FA, half, groups=None):
            for c4 in (range(CS // 4) if groups is None else groups):
                psA = P.psum("tmp")
                for cc in range(4):
                    c = c4 * 4 + cc
                    lhs = inp_fn(c)
                    for vi, l_ in enumerate(lhs):
                        P.op("pe", lambda: nc.tensor.matmul(psA[:, cc * 128:(cc + 1) * 128], lhsT=l_, rhs=FA[:K, 0, vi, half, :],
                                                            start=(vi == 0), stop=(vi == len(lhs) - 1)),
                             r=[in_res_fn(c), FA.res], w=[psA.res])
                k = cn["g"]
                cn["g"] += 1
                a_ = WK[k % 2]
                m_ = WK[2 + k % 2]
                P.op("act", lambda: nc.scalar.copy(out=a_[:, 0, :], in_=psA[:, :]), r=[psA.res], w=[a_.res])
                en = "pool" if c4 % 3 == 2 else "dve"
                eo = P.engs[en]
                for j in range(2):
                    P.op(en, lambda: eo.tensor_tensor(out=m_[:, j, :].rearrange("p (a k) -> p a k", k=64),
                                                      in0=a_[:, 0, :].rearrange("p (a k) -> p a k", k=64),
                                                      in1=twb[:, j, half, 0:64].unsqueeze(1).to_broadcast([128, 8, 64]), op=ALU.mult),
                         r=[a_.res, twb.res], w=[m_.res])
                m1 = m_[:, 0, :].rearrange("p (cc r k) -> p cc r k", cc=4, r=2)
                m2 = m_[:, 1, :].rearrange("p (cc r k) -> p cc r k", cc=4, r=2)
                P.op(en, lambda: eo.tensor_tensor(out=Abuf[:, 0, c4 * 4:c4 * 4 + 4, :], in0=m1[:, :, 0, :], in1=m2[:, :, 1, :], op=ALU.subtract),
                     r=[m_.res], w=[AbR[c4 // 2]])
                P.op(en, lambda: eo.tensor_tensor(out=Abuf[:, 1, c4 * 4:c4 * 4 + 4, :], in0=m1[:, :, 1, :], in1=m2[:, :, 0, :], op=ALU.add),
                     r=[m_.res], w=[AbR[c4 // 2]])

        def stage_b(DF, M, half, want_imag, evac, blocks=None):
            for cb in (range(CS // 8) if blocks is None else blocks):
                ar = Abuf[:, 0, cb * 8:(cb + 1) * 8, :]
                ai = Abuf[:, 1, cb * 8:(cb + 1) * 8, :]
                psR = P.psum("acc")
                P.op("pe", lambda: nc.tensor.matmul(psR[:M, :], lhsT=DF[:, 0, :M], rhs=ar, start=True, stop=False),
                     r=[DF.res, AbR[cb]], w=[psR.res])
                P.op("pe", lambda: nc.tensor.matmul(psR[:M, :], lhsT=DF[:, 2, :M], rhs=ai, start=False, stop=True),
                     r=[DF.res, AbR[cb]], w=[psR.res])
                psI = None
                if want_imag:
                    psI = P.psum("acc")
                    P.op("pe", lambda: nc.tensor.matmul(psI[:M, :], lhsT=DF[:, 1, :M], rhs=ar, start=True, stop=False),
                         r=[DF.res, AbR[cb]], w=[psI.res])
                    P.op("pe", lambda: nc.tensor.matmul(psI[:M, :], lhsT=DF[:, 0, :M], rhs=ai, start=False, stop=True),
                         r=[DF.res, AbR[cb]], w=[psI.res])
                evac(psR, psI, cb, half)

        def fft_pass(inp_fn, in_res_fn, K, FA, DF, M, want_imag, evac):
            stage_a(inp_fn, in_res_fn, K, FA, 0)
            for cb in range(CS // 8):
                stage_b(DF, M, 0, want_imag, evac, blocks=[cb])
                stage_a(inp_fn, in_res_fn, K, FA, 1, groups=[2 * cb, 2 * cb + 1])
            stage_b(DF, M, 1, want_imag, evac)

        INVN = 1.0 / NFFT

        def blk3(ps, M=128):
            return ps[:M, :].rearrange("p (c k) -> p c k", k=64)

        def evac_filt_first(psR, psI, cb, half):
            ks = slice(half * 64, (half + 1) * 64)
            cs_ = slice(cb * 8, (cb + 1) * 8)
            P.op("act", lambda: nc.scalar.mul(out=Kf[:, 0, cs_, ks], in_=blk3(psR), mul=INVN), r=[psR.res], w=[KfR[cb]])
            P.op("act", lambda: nc.scalar.mul(out=Kf[:, 1, cs_, ks], in_=blk3(psI), mul=-INVN), r=[psI.res], w=[KfR[cb]])

        def evac_filt_second(psR, psI, cb, half):
            ks = slice(half * 64, (half + 1) * 64)
            cs_ = slice(cb * 8, (cb + 1) * 8)
            for ri, ps_ in ((0, psR), (1, psI)):
                P.op("dve", lambda: nc.vector.scalar_tensor_tensor(out=Kf[:, ri, cs_, ks], in0=blk3(ps_), scalar=INVN, in1=Kf[:, ri, cs_, ks],
                                                                   op0=ALU.mult, op1=ALU.add), r=[ps_.res, KfR[cb]], w=[KfR[cb]])

        def evac_pointwise(psR, psI, cb, half):
            ks = slice(half * 64, (half + 1) * 64)
            cs_ = slice(cb * 8, (cb + 1) * 8)
            k = cn["e"]
            cn["e"] += 1
            x_ = WK[k % 2]
            ta, tb = WK[2], WK[3]
            P.op("act", lambda: nc.scalar.copy(out=x_[:, 0, :], in_=psR[:, :]), r=[psR.res], w=[x_.res])
            P.op("act", lambda: nc.scalar.copy(out=x_[:, 1, :], in_=psI[:, :]), r=[psI.res], w=[x_.res])
            v3 = lambda ap_: ap_.rearrange("p (c k) -> p c k", k=64)
            kr, nki = Kf[:, 0, cs_, ks], Kf[:, 1, cs_, ks]
            P.op("dve", lambda: nc.vector.tensor_tensor(out=v3(ta[:, 0, :]), in0=v3(x_[:, 0, :]), in1=kr, op=ALU.mult), r=[x_.res, KfR[cb]], w=[ta.res])
            P.op("dve", lambda: nc.vector.tensor_tensor(out=v3(ta[:, 1, :]), in0=v3(x_[:, 1, :]), in1=nki, op=ALU.mult), r=[x_.res, KfR[cb]], w=[ta.res])
            P.op("dve", lambda: nc.vector.tensor_tensor(out=Ybuf[:, 0, cs_, ks], in0=v3(ta[:, 0, :]), in1=v3(ta[:, 1, :]), op=ALU.add),
                 r=[ta.res], w=[YbR[cb]])
            P.op("dve", lambda: nc.vector.tensor_tensor(out=v3(tb[:, 0, :]), in0=v3(x_[:, 0, :]), in1=nki, op=ALU.mult), r=[x_.res, KfR[cb]], w=[tb.res])
            P.op("dve", lambda: nc.vector.tensor_tensor(out=v3(tb[:, 1, :]), in0=v3(x_[:, 1, :]), in1=kr, op=ALU.mult), r=[x_.res, KfR[cb]], w=[tb.res])
            P.op("dve", lambda: nc.vector.tensor_tensor(out=Ybuf[:, 1, cs_, ks], in0=v3(tb[:, 0, :]), in1=v3(tb[:, 1, :]), op=ALU.subtract),
                 r=[tb.res], w=[YbR[cb]])

        def make_evac_inv(M):
            def ev(psR, psI, cb, half):
                o_ = Xin[:M, cb * 8:(cb + 1) * 8, half * 64:(half + 1) * 64]
                if cb % 2 == 0:
                    P.op("act", lambda: nc.scalar.copy(out=o_, in_=blk3(psR, M)), r=[psR.res], w=[Xin.res])
                else:
                    P.op("dve", lambda: nc.vector.tensor_copy(out=o_, in_=blk3(psR, M)), r=[psR.res], w=[Xin.res])
            return ev

        def yin(c):
            return [Ybuf[:, 0, c, :], Ybuf[:, 1, c, :]]

        def yin_res(c):
            return YbR[c // 8]

        def xin_rows(K):
            return lambda c: [Xin[:K, c, :]]

        def xin_res(c):
            return Xin.res

        for g in range(4):
            short_conv(zhyT[g], g, vc, 8)
            short_conv(zhyT[4 + g], 4 + g, x1c, 8)
            short_conv(zhyT[8 + g], 8 + g, x2c, 2)
            for sub in range(2):
                cs = sub * 64
                c0abs = g * 128 + cs
                for hh_ in range(2):
                    P.dma("sp", dec0[hh_ * 64:(hh_ + 1) * 64, :], t_dec0[c0abs:c0abs + 64, :], w=[dec0.res])
                    P.dma("sp", decs[hh_ * 64:(hh_ + 1) * 64, :], t_decs[c0abs:c0abs + 64, :], w=[decs.res])
                for o in range(2):
                    filter_gen(c0abs, o)
                    to_xin_filter()
                    fft_pass(xin_rows(128), xin_res, 128, FAf, DFu, 128, True, evac_filt_first)
                    src = vc if o == 0 else y1
                    M = 64 if o == 0 else 16
                    to_xin(src, cs)
                    fft_pass(xin_rows(64), xin_res, 64, FAq, DFu, 128, True, evac_pointwise)
                    fft_pass(yin, yin_res, 128, FAu, DFq, M, False, make_evac_inv(M))
                    if o == 0:
                        for g16 in range(8):
                            ps = P.psum("tmp")
                            pbf = ps.t.bitcast(BF16)
                            for s_ in range(16):
                                lo = g16 * 16 + s_
                                P.op("pe", lambda: nc.tensor.transpose(out=pbf[cs:cs + 64, s_ * 64:(s_ + 1) * 64], in_=Xin[:64, :, lo],
                                                                       identity=ident[0:64, 0:64]), r=[Xin.res, ident.res], w=[ps.res])
                            gt_ = gt[0]
                            vw = lambda X_: X_[cs:cs + 64, :].rearrange("p (t l) -> p l t", l=128)[:, g16 * 16:(g16 + 1) * 16, :]
                            gv = gt_[cs:cs + 64, :].rearrange("p (l t) -> p l t", t=64)
                            P.op("dve", lambda: nc.vector.tensor_scalar(out=gv, in0=vw(vc), scalar1=skipc[cs:cs + 64, 0, g:g + 1], scalar2=None, op0=ALU.mult),
                                 r=[vc.res, skipc.res], w=[gt_.res])
                            P.op("dve", lambda: nc.vector.scalar_tensor_tensor(out=gv, in0=pbf[cs:cs + 64, 0:1024].rearrange("p (l t) -> p l t", t=64),
                                                                               scalar=rtot[cs:cs + 64, 0:1], in1=gv, op0=ALU.mult, op1=ALU.add),
                                 r=[ps.res, rtot.res, gt_.res], w=[gt_.res])
                            P.op("dve", lambda: nc.vector.tensor_tensor(out=vw(y1), in0=gv, in1=vw(x1c), op=ALU.mult),
                                 r=[gt_.res, x1c.res], w=[y1.res])
                    else:
                        for g64 in range(2):
                            ps = P.psum("tmp")
                            pbf = ps.t.bitcast(BF16)
                            for s_ in range(64):
                                lo = g64 * 64 + s_
                                P.op("pe", lambda: nc.tensor.transpose(out=pbf[cs:cs + 64, s_ * 16:(s_ + 1) * 16], in_=Xin[:16, :, lo],
                                                                       identity=ident[0:16, 0:16]), r=[Xin.res, ident.res], w=[ps.res])
                            gt_ = gt[0]
                            vw = lambda X_: X_[cs:cs + 64, 0:2048].rearrange("p (t l) -> p l t", l=128)[:, g64 * 64:(g64 + 1) * 64, :]
                            gv = gt_[cs:cs + 64, :].rearrange("p (l t) -> p l t", t=16)
                            P.op("dve", lambda: nc.vector.tensor_scalar(out=gv, in0=vw(y1), scalar1=skipc[cs:cs + 64, 1, g:g + 1], scalar2=None, op0=ALU.mult),
                                 r=[y1.res, skipc.res], w=[gt_.res])
                            P.op("dve", lambda: nc.vector.scalar_tensor_tensor(out=gv, in0=pbf[cs:cs + 64, 0:1024].rearrange("p (l t) -> p l t", t=16),
                                                                               scalar=rtot[cs:cs + 64, 1:2], in1=gv, op0=ALU.mult, op1=ALU.add),
                                 r=[ps.res, rtot.res, gt_.res], w=[gt_.res])
                            P.op("dve", lambda: nc.vector.tensor_tensor(out=vw(x2c), in0=gv, in1=vw(x2c), op=ALU.mult),
                                 r=[gt_.res, x2c.res], w=[x2c.res])
            P.dma("pool", hyTd[g].t.ap(), x2c[:], r=[x2c.res], w=[hyTd[g].res])
            if "hy1" in debug and g == 0:
                d1 = dbg_out("y1", [128, L], BF16)
                d2 = dbg_out("vc", [128, L], BF16)
                d3 = dbg_out("Kf", [128, 2 * 128 * CS], BF16)
                P.dma("sp", d1[:, :], y1[:], r=[y1.res], w=[d1.res])
                P.dma("sp", d2[:, :], vc[:], r=[vc.res], w=[d2.res])
                P.dma("sp", d3[:, :], Kf[:].rearrange("p r c k -> p (r c k)"), r=KfR, w=[d3.res])
            if "stopD1" in debug:
                break
        P.barrier()

    if "phaseD" in debug:
        d1 = dbg_out("hyT", [512, 2048], BF16)
        for g in range(4):
            P.dma("sp", d1[g * 128:(g + 1) * 128, :], hyTd[g].t.ap(), r=[hyTd[g].res], w=[d1.res])
    if "stopD" in debug or "stopD1" in debug:
        P.barrier()
        return nc, es, dbg

    x1d = P.dram("x1d", [2048, D], F32)
    h2T = P.sb(es, "h2T", [128, 8, 2048], BF16)
    with ExitStack() as pe_:
        whb = P.sb(pe_, "whb", [128, 4, D], BF16)
        wnb = P.sb(pe_, "wnb", [128, 4, D], BF16)
        wob = P.sb(pe_, "wob", [128, 8, D], BF16)
        with ExitStack() as pst:
            wstg = P.sb(pst, "wstg", [128, 4, D], F32)
            P.dma("sp", wstg[:], w_hy_out[:, :].rearrange("(kc p) n -> p kc n", p=128), w=[wstg.res])
            P.op("dve", lambda: nc.vector.tensor_copy(out=whb[:], in_=wstg[:]), r=[wstg.res], w=[whb.res])
            P.dma("sp", wstg[:], w_na_out[:, :].rearrange("(kc p) n -> p kc n", p=128), w=[wstg.res])
            P.op("pool", lambda: nc.gpsimd.tensor_copy(out=wnb[:], in_=wstg[:]), r=[wstg.res], w=[wnb.res])
            for hh in range(2):
                P.dma("sp", wstg[:], w_out[hh * 512:(hh + 1) * 512, :].rearrange("(kc p) n -> p kc n", p=128), w=[wstg.res])
                P.op("dve", lambda: nc.vector.tensor_copy(out=wob[:, hh * 4:(hh + 1) * 4, :], in_=wstg[:]), r=[wstg.res], w=[wob.res])
            P.barrier()
        hyb = P.sb(pe_, "hyb", [128, 4, 512], BF16)
        atb = P.sb(pe_, "atb", [128, 4, 512], BF16)
        gtb = P.sb(pe_, "gtb", [128, 16, 512], BF16)
        mrg = P.sb(pe_, "mrg", [128, 8, 512], BF16)
        m1 = [P.sb(pe_, "m1_%d" % i, [128, 512], F32) for i in range(2)]
        m2 = [P.sb(pe_, "m2_%d" % i, [128, 512], F32) for i in range(2)]
        xe = [P.sb(pe_, "xe%d" % i, [128, D], F32) for i in range(2)]
        x1t = [P.sb(pe_, "x1t%d" % i, [128, D], F32) for i in range(2)]
        xn2 = [P.sb(pe_, "xn2_%d" % i, [128, D], BF16) for i in range(2)]
        junk2 = P.sb(pe_, "junk2", [128, D], BF16)
        se = [P.sb(pe_, "se%d" % i, [128, 4], F32) for i in range(2)]
        for blk in range(4):
            tcols = slice(blk * 512, (blk + 1) * 512)
            for g in range(4):
                P.dma("sp", hyb[:, g, :], hyTd[g][:, tcols], r=[hyTd[g].res], w=[hyb.res])
            P.dma("sp", atb[:], attTd[:, :, tcols], r=[attTd.res], w=[atb.res])
            for m in range(16):
                P.dma("sp", gtb[:, m, :], gsT[m][:, tcols], r=[gsT[m].res], w=[gtb.res])
            for fc in range(8):
                fs = slice(fc * 128, (fc + 1) * 128)
                psH = P.psum("acc")
                psA = P.psum("acc")
                for kc in range(4):
                    P.op("pe", lambda: nc.tensor.matmul(psH[:, :], lhsT=whb[:, kc, fs], rhs=hyb[:, kc, :], start=(kc == 0), stop=(kc == 3)),
                         r=[whb.res, hyb.res], w=[psH.res])
                for kc in range(4):
                    P.op("pe", lambda: nc.tensor.matmul(psA[:, :], lhsT=wnb[:, kc, fs], rhs=atb[:, kc, :], start=(kc == 0), stop=(kc == 3)),
                         r=[wnb.res, atb.res], w=[psA.res])
                a_, b_ = m1[fc % 2], m2[fc % 2]
                P.op("dve", lambda: nc.vector.tensor_tensor(out=a_[:], in0=psH[:, :], in1=gtb[:, fc, :], op=ALU.mult), r=[psH.res, gtb.res], w=[a_.res])
                P.op("dve", lambda: nc.vector.tensor_tensor(out=b_[:], in0=psA[:, :], in1=gtb[:, 8 + fc, :], op=ALU.mult), r=[psA.res, gtb.res], w=[b_.res])
                P.op("pool", lambda: nc.gpsimd.tensor_tensor(out=mrg[:, fc, :], in0=a_[:], in1=b_[:], op=ALU.add), r=[a_.res, b_.res], w=[mrg.res])
            for tt in range(4):
                i = blk * 4 + tt
                x_ = xe[i % 2]
                x1_ = x1t[i % 2]
                xn_ = xn2[i % 2]
                s_ = se[i % 2]
                P.dma("sp", x_[:], xroll[i * 128:(i + 1) * 128, :], w=[x_.res])
                for hh in range(2):
                    hs = slice(hh * 512, (hh + 1) * 512)
                    ps = P.psum("tmp")
                    for fc in range(8):
                        P.op("pe", lambda: nc.tensor.matmul(ps[:, :], lhsT=mrg[:, fc, tt * 128:(tt + 1) * 128], rhs=wob[:, fc, hs],
                                                            start=(fc == 0), stop=(fc == 7)), r=[mrg.res, wob.res], w=[ps.res])
                    P.op("dve", lambda: nc.vector.tensor_tensor(out=x1_[:, hs], in0=ps[:, :], in1=grow[:, 0, hs], op=ALU.mult),
                         r=[ps.res, grow.res], w=[x1_.res])
                    P.op("pool", lambda: nc.gpsimd.tensor_tensor(out=x1_[:, hs], in0=x1_[:, hs], in1=x_[:, hs], op=ALU.add),
                         r=[x1_.res, x_.res], w=[x1_.res])
                P.dma("pool", x1d[i * 128:(i + 1) * 128, :], x1_[:], r=[x1_.res], w=[x1d.res])
                P.op("act", lambda: nc.scalar.activation(out=junk2[:], in_=x1_[:], func=AF.Square, accum_out=s_[:, 0:1]),
                     r=[x1_.res], w=[junk2.res, s_.res])
                P.op("dve", lambda: nc.vector.tensor_scalar(out=s_[:, 1:2], in0=s_[:, 0:1], scalar1=1.0 / D, scalar2=EPS,
                                                            op0=ALU.mult, op1=ALU.add), r=[s_.res], w=[s_.res])
                P.op("act", lambda: nc.scalar.activation(out=s_[:, 2:3], in_=s_[:, 1:2], func=AF.Sqrt), r=[s_.res], w=[s_.res])
                P.op("dve", lambda: nc.vector.reciprocal(out=s_[:, 3:4], in_=s_[:, 2:3]), r=[s_.res], w=[s_.res])
                P.op("dve", lambda: nc.vector.tensor_scalar(out=xn_[:], in0=x1_[:], scalar1=s_[:, 3:4], scalar2=None, op0=ALU.mult),
                     r=[x1_.res, s_.res], w=[xn_.res])
                pb_ = P.psum("tmp")
                pbf = pb_.t.bitcast(BF16)
                for kc in range(8):
                    P.op("pe", lambda: nc.tensor.transpose(out=pbf[:, kc * 128:(kc + 1) * 128], in_=xn_[:, kc * 128:(kc + 1) * 128],
                                                           identity=ident[:]), r=[xn_.res, ident.res], w=[pb_.res])
                for kc in range(8):
                    P.op("act", lambda: nc.scalar.activation(out=h2T[:, kc, i * 128:(i + 1) * 128], in_=pbf[:, kc * 128:(kc + 1) * 128],
                                                             func=AF.Identity, scale=mods[:, MI_G2, kc:kc + 1], bias=mods[:, MI_S2, kc:kc + 1]),
                         r=[pb_.res, mods.res], w=[h2T.res])
        P.barrier()

    if "phaseE" in debug:
        d1 = dbg_out("x1", [2048, D])
        d2 = dbg_out("h2T", [128, 8 * 2048], BF16)
        P.dma("sp", d1[:, :], x1d.t.ap(), r=[x1d.res], w=[d1.res])
        P.dma("sp", d2[:, :], h2T[:].rearrange("p k t -> p (k t)"), r=[h2T.res], w=[d2.res])
    if "stopE" in debug:
        P.barrier()
        return nc, es, dbg

    I1T = P.sb(es, "I1T", [128, 2048], F32)
    I2T = P.sb(es, "I2T", [128, 2048], F32)
    GWT = P.sb(es, "GWT", [128, 2048], F32)
    io128 = P.sb(es, "io128", [128, 128], F32)
    io16 = P.sb(es, "io16", [128, 16], F32)
    P.dma("sp", io128[:], t_io128[:, :], w=[io128.res])
    P.dma("sp", io16[:], t_io128[:, 0:16], w=[io16.res])
    with ExitStack() as pf1:
        wqb = P.sb(pf1, "wqb", [128, 8, 2048], BF16)
        wqs = [P.sb(pf1, "wqs%d" % i, [128, 8, 256], F32) for i in range(2)]
        keyb = P.sb(pf1, "keyb", [128, 16, 128], BF16)
        keyf = P.sb(pf1, "keyf", [128, 16, 128], F32)
        P.dma("sp", keyf[:], peer_keysT[:, :, :], w=[keyf.res])
        P.op("pool", lambda: nc.gpsimd.tensor_copy(out=keyb[:], in_=keyf[:]), r=[keyf.res], w=[keyb.res])
        for pc in range(8):
            ws = wqs[pc % 2]
            P.dma("sp", ws[:], peer_w_q[:, pc * 256:(pc + 1) * 256].rearrange("(kc p) n -> p kc n", p=128), w=[ws.res])
            eng = "dve" if pc % 2 == 0 else "pool"
            P.op(eng, lambda: P.engs[eng].tensor_copy(out=wqb[:, :, pc * 256:(pc + 1) * 256], in_=ws[:]), r=[ws.res], w=[wqb.res])
        qpT = P.sb(pf1, "qpT", [128, 16, 512], BF16)
        sc = P.sb(pf1, "sc", [128, 16, 128], F32)
        sc2 = P.sb(pf1, "sc2", [128, 16, 128], F32)
        top = P.sb(pf1, "top", [128, 16, 16], F32)
        idxu = P.sb(pf1, "idxu", [128, 16, 16], U32)
        idxf = P.sb(pf1, "idxf", [128, 16, 16], F32)
        cand = P.sb(pf1, "cand", [128, 8, 256], F32)
        cand2 = P.sb(pf1, "cand2", [128, 8, 256], F32)
        best = P.sb(pf1, "best", [128, 8, 16], F32)
        posu = P.sb(pf1, "posu", [128, 8, 16], U32)
        pos2 = P.sb(pf1, "pos2", [128, 2, 128], U32)
        posf = P.sb(pf1, "posf", [128, 2, 128], F32)
        eq = P.sb(pf1, "eq", [128, 128, 16], F32)
        sm = P.sb(pf1, "sm", [128, 4, 128], F32)
        ssum = P.sb(pf1, "ssum", [128, 2, 8], F32)
        for blk in range(4):
            tcols = slice(blk * 512, (blk + 1) * 512)
            for hp in range(16):
                ps = P.psum("tmp")
                for kc in range(8):
                    P.op("pe", lambda: nc.tensor.matmul(ps[:, :], lhsT=wqb[:, kc, hp * 128:(hp + 1) * 128], rhs=h2T[:, kc, tcols],
                                                        start=(kc == 0), stop=(kc == 7)), r=[wqb.res, h2T.res], w=[ps.res])
                if hp % 2 == 0:
                    P.op("act", lambda: nc.scalar.copy(out=qpT[:, hp, :], in_=ps[:, :]), r=[ps.res], w=[qpT.res])
                else:
                    P.op("dve", lambda: nc.vector.tensor_copy(out=qpT[:, hp, :], in_=ps[:, :]), r=[ps.res], w=[qpT.res])
            for tt in range(4):
                i = blk * 4 + tt
                for h4 in range(4):
                    ps = P.psum("tmp")
                    for j in range(4):
                        hp = h4 * 4 + j
                        P.op("pe", lambda: nc.tensor.matmul(ps[:, j * 128:(j + 1) * 128], lhsT=qpT[:, hp, tt * 128:(tt + 1) * 128],
                                                            rhs=keyb[:, hp, :], start=True, stop=True), r=[qpT.res, keyb.res], w=[ps.res])
                    P.op("act", lambda: nc.scalar.copy(out=sc[:, h4 * 4:(h4 + 1) * 4, :], in_=ps[:, :].rearrange("p (j n) -> p j n", n=128)),
                         r=[ps.res], w=[sc.res])
                for hp in range(16):
                    P.op("dve", lambda: nc.vector.max(out=top[:, hp, 0:8], in_=sc[:, hp, :]), r=[sc.res], w=[top.res])
                    P.op("dve", lambda: nc.vector.max_index(out=idxu[:, hp, 0:8], in_max=top[:, hp, 0:8], in_values=sc[:, hp, :]),
                         r=[sc.res, top.res], w=[idxu.res])
                    P.op("dve", lambda: nc.vector.match_replace(out=sc2[:, hp, :], in_to_replace=top[:, hp, 0:8], in_values=sc[:, hp, :],
                                                                imm_value=-1e30), r=[sc.res, top.res], w=[sc2.res])
                    P.op("dve", lambda: nc.vector.max(out=top[:, hp, 8:16], in_=sc2[:, hp, :]), r=[sc2.res], w=[top.res])
                    P.op("dve", lambda: nc.vector.max_index(out=idxu[:, hp, 8:16], in_max=top[:, hp, 8:16], in_values=sc2[:, hp, :]),
                         r=[sc2.res, top.res], w=[idxu.res])
                P.op("dve", lambda: nc.vector.tensor_copy(out=idxf[:], in_=idxu[:]), r=[idxu.res], w=[idxf.res])
                top4 = top[:].rearrange("p (h t) k -> p h t k", t=2)
                idx4 = idxf[:].rearrange("p (h t) k -> p h t k", t=2)
                P.op("dve", lambda: nc.vector.tensor_tensor(out=cand[:].rearrange("p h (a b) -> p h a b", b=16),
                                                            in0=top4[:, :, 0, :].unsqueeze(3).to_broadcast([128, 8, 16, 16]),
                                                            in1=top4[:, :, 1, :].unsqueeze(2).to_broadcast([128, 8, 16, 16]), op=ALU.add),
                     r=[top.res], w=[cand.res])
                for h in range(8):
                    P.op("dve", lambda: nc.vector.max(out=best[:, h, 0:8], in_=cand[:, h, :]), r=[cand.res], w=[best.res])
                    P.op("dve", lambda: nc.vector.max_index(out=posu[:, h, 0:8], in_max=best[:, h, 0:8], in_values=cand[:, h, :]),
                         r=[cand.res, best.res], w=[posu.res])
                    P.op("dve", lambda: nc.vector.match_replace(out=cand2[:, h, :], in_to_replace=best[:, h, 0:8], in_values=cand[:, h, :],
                                                                imm_value=-1e30), r=[cand.res, best.res], w=[cand2.res])
                    P.op("dve", lambda: nc.vector.max(out=best[:, h, 8:16], in_=cand2[:, h, :]), r=[cand2.res], w=[best.res])
                    P.op("dve", lambda: nc.vector.max_index(out=posu[:, h, 8:16], in_max=best[:, h, 8:16], in_values=cand2[:, h, :]),
                         r=[cand2.res, best.res], w=[posu.res])
                sm3 = lambda k_: sm[:, k_, :].rearrange("p (h k) -> p h k", k=16)
                P.op("dve", lambda: nc.vector.tensor_tensor(out=sm3(0), in0=best[:], in1=best[:, :, 0:1].to_broadcast([128, 8, 16]), op=ALU.subtract),
                     r=[best.res], w=[sm.res])
                P.op("act", lambda: nc.scalar.activation(out=sm[:, 0, :], in_=sm[:, 0, :], func=AF.Exp), r=[sm.res], w=[sm.res])
                P.op("dve", lambda: nc.vector.tensor_reduce(out=ssum[:, 0, :], in_=sm3(0), axis=AX.X, op=ALU.add), r=[sm.res], w=[ssum.res])
                P.op("dve", lambda: nc.vector.reciprocal(out=ssum[:, 1, :], in_=ssum[:, 0, :]), r=[ssum.res], w=[ssum.res])
                P.op("dve", lambda: nc.vector.tensor_tensor(out=sm3(1), in0=sm3(0), in1=ssum[:, 1, :].unsqueeze(2).to_broadcast([128, 8, 16]), op=ALU.mult),
                     r=[sm.res, ssum.res], w=[sm.res])
                posv = posu[:].rearrange("p h k -> p (h k)")
                P.op("dve", lambda: nc.vector.tensor_single_scalar(out=pos2[:, 0, :], in_=posv, scalar=4, op=ALU.logical_shift_right),
                     r=[posu.res], w=[pos2.res])
                P.op("dve", lambda: nc.vector.tensor_single_scalar(out=pos2[:, 1, :], in_=posv, scalar=15, op=ALU.bitwise_and),
                     r=[posu.res], w=[pos2.res])
                P.op("dve", lambda: nc.vector.tensor_copy(out=posf[:], in_=pos2[:]), r=[pos2.res], w=[posf.res])
                for t_ in range(2):
                    P.op("dve", lambda: nc.vector.tensor_tensor(out=eq[:], in0=posf[:, t_, :].unsqueeze(2).to_broadcast([128, 128, 16]),
                                                                in1=io16[:].unsqueeze(1).to_broadcast([128, 128, 16]), op=ALU.is_equal),
                         r=[posf.res, io16.res], w=[eq.res])
                    eq4 = eq[:].rearrange("p (h k) c -> p h k c", k=16)
                    P.op("dve", lambda: nc.vector.tensor_tensor(out=eq4, in0=eq4, in1=idx4[:, :, t_, :].unsqueeze(2).to_broadcast([128, 8, 16, 16]),
                                                                op=ALU.mult), r=[eq.res, idxf.res], w=[eq.res])
                    P.op("dve", lambda: nc.vector.tensor_reduce(out=sm[:, 2 + t_, :], in_=eq[:], axis=AX.X, op=ALU.add), r=[eq.res], w=[sm.res])
                for k_, dstT in ((2, I1T), (3, I2T), (1, GWT)):
                    ps = P.psum("tmp")
                    P.op("pe", lambda: nc.tensor.transpose(out=ps[:, 0:128], in_=sm[:, k_, :], identity=identf[:]), r=[sm.res, identf.res], w=[ps.res])
                    P.op("act", lambda: nc.scalar.copy(out=dstT[:, i * 128:(i + 1) * 128], in_=ps[:, 0:128]), r=[ps.res], w=[dstT.res])
        P.barrier()

    if "phaseF1" in debug:
        d1 = dbg_out("I1T", [128, 2048])
        d2 = dbg_out("I2T", [128, 2048])
        d3 = dbg_out("GWT", [128, 2048])
        P.dma("sp", d1[:, :], I1T[:], r=[I1T.res], w=[d1.res])
        P.dma("sp", d2[:, :], I2T[:], r=[I2T.res], w=[d2.res])
        P.dma("sp", d3[:, :], GWT[:], r=[GWT.res], w=[d3.res])
    if "stopF1" in debug:
        P.barrier()
        return nc, es, dbg

    with ExitStack() as pf2:
        GTb = P.sb(pf2, "GTb", [128, 128, 256], BF16)
        TC = 16
        O2 = P.sb(pf2, "O2", [128, TC, 128], BF16)
        O1e = P.sb(pf2, "O1e", [128, TC, 128], BF16)
        O1 = P.sb(pf2, "O1", [128, TC, 128], BF16)
        ubt = [P.sb(pf2, "ubt%d" % i, [128, 4, 8, 128], BF16) for i in range(3)]
        vbt = [P.sb(pf2, "vbt%d" % i, [128, 4, D], BF16) for i in range(3)]
        Ag = [P.sb(pf2, "Ag%d" % i, [128, 256], BF16) for i in range(3)]
        AG = [P.sb(pf2, "AG%d" % i, [128, 256], BF16) for i in range(3)]
        x1f = [P.sb(pf2, "x1f%d" % i, [128, D], F32) for i in range(1)]
        of_ = [P.sb(pf2, "of%d" % i, [128, D], F32) for i in range(2)]
        for b8 in range(8):
            t0 = b8 * 256
            for ch in range(256 // TC):
                c0 = t0 + ch * TC
                P.op("dve", lambda: nc.vector.tensor_tensor(out=O2[:], in0=io128[:].unsqueeze(1).to_broadcast([128, TC, 128]),
                                                            in1=I2T[:, c0:c0 + TC].unsqueeze(2).to_broadcast([128, TC, 128]), op=ALU.is_equal),
                     r=[io128.res, I2T.res], w=[O2.res])
                P.op("dve", lambda: nc.vector.tensor_tensor(out=O1e[:], in0=io128[:].unsqueeze(1).to_broadcast([128, TC, 128]),
                                                            in1=I1T[:, c0:c0 + TC].unsqueeze(2).to_broadcast([128, TC, 128]), op=ALU.is_equal),
                     r=[io128.res, I1T.res], w=[O1e.res])
                P.op("pool", lambda: nc.gpsimd.tensor_tensor(out=O1[:], in0=O1e[:], in1=GWT[:, c0:c0 + TC].unsqueeze(2).to_broadcast([128, TC, 128]),
                                                             op=ALU.mult), r=[O1e.res, GWT.res], w=[O1.res])
                for t4 in range(TC // 4):
                    ps = P.psum("tmp")
                    for j in range(4):
                        t_ = t4 * 4 + j
                        P.op("pe", lambda: nc.tensor.matmul(ps[:, j * 128:(j + 1) * 128], lhsT=O1[:, t_, :], rhs=O2[:, t_, :], start=True, stop=True),
                             r=[O1.res, O2.res], w=[ps.res])
                    o_ = GTb[:, :, ch * TC + t4 * 4:ch * TC + t4 * 4 + 4]
                    i_ = ps[:, :].rearrange("p (t j) -> p j t", j=128)
                    if t4 % 2 == 0:
                        P.op("act", lambda: nc.scalar.copy(out=o_, in_=i_), r=[ps.res], w=[GTb.res])
                    else:
                        P.op("dve", lambda: nc.vector.tensor_copy(out=o_, in_=i_), r=[ps.res], w=[GTb.res])
            accs = [[P.psum("acc") for _ in range(2)] for _ in range(2)]
            def s_stage(jp):
                u_ = ubt[(jp // 4) % 3]
                v_ = vbt[(jp // 4) % 3]
                j4 = jp % 4
                if j4 == 0:
                    P.dma("sp", u_[:], ubd[jp // 4, :, :, :, :], r=[ubd_res], w=[u_.res])
                    P.dma("sp", v_[:], vbd[jp // 4, :, :, :], r=[vbd_res], w=[v_.res])
                psS = P.psum("tmp")
                for kc in range(8):
                    P.op("pe", lambda: nc.tensor.matmul(psS[:, 0:256], lhsT=u_[:, j4, kc, :], rhs=h2T[:, kc, t0:t0 + 256],
                                                        start=(kc == 0), stop=(kc == 7)), r=[u_.res, h2T.res], w=[psS.res])
                a_ = Ag[jp % 3]
                g_ = AG[jp % 3]
                P.op("act", lambda: nc.scalar.activation(out=a_[:], in_=psS[:, 0:256], func=AF.Gelu), r=[psS.res], w=[a_.res])
                P.op("dve", lambda: nc.vector.tensor_tensor(out=g_[:], in0=a_[:], in1=GTb[:, jp, :], op=ALU.mult), r=[a_.res, GTb.res], w=[g_.res])
                return g_, v_, j4

            def acc_stage(jp, st_):
                g_, v_, j4 = st_
                for tt in range(2):
                    for hh in range(2):
                        P.op("pe", lambda: nc.tensor.matmul(accs[tt][hh][:, :], lhsT=g_[:, tt * 128:(tt + 1) * 128], rhs=v_[:, j4, hh * 512:(hh + 1) * 512],
                                                            start=(jp == 0), stop=(jp == 127)), r=[g_.res, v_.res], w=[accs[tt][hh].res])

            prev = None
            for jp in range(128):
                cur = s_stage(jp)
                if prev is not None:
                    acc_stage(jp - 1, prev)
                prev = cur
            acc_stage(127, prev)
            for tt in range(2):
                i = b8 * 2 + tt
                xf = x1f[0]
                o_ = of_[tt]
                P.dma("sp", xf[:], x1d[i * 128:(i + 1) * 128, :], r=[x1d.res], w=[xf.res])
                for hh in range(2):
                    hs = slice(hh * 512, (hh + 1) * 512)
                    P.op("dve", lambda: nc.vector.tensor_tensor(out=o_[:, hs], in0=accs[tt][hh][:, :], in1=grow[:, 1, hs], op=ALU.mult),
                         r=[accs[tt][hh].res, grow.res], w=[o_.res])
                    P.op("pool", lambda: nc.gpsimd.tensor_tensor(out=o_[:, hs], in0=o_[:, hs], in1=xf[:, hs], op=ALU.add),
                         r=[o_.res, xf.res], w=[o_.res])
                P.dma("pool", out[i * 128:(i + 1) * 128, :], o_[:], r=[o_.res], w=[out.res])
        P.barrier()

    P.barrier()
    return nc, es, dbg


def _colT(v, nch):
    return np.ascontiguousarray(np.asarray(v, np.float32).reshape(nch, 128).T)


def prep_inputs(inp):
    f = lambda a: np.ascontiguousarray(np.asarray(a, np.float32))
    x, c, ctx, c_ctx = f(inp["x"]), f(inp["c"]), f(inp["ctx"]), f(inp["c_ctx"])
    w_ada = f(inp["w_ada"][0])
    b_ada = f(inp["b_ada"][0])
    shared = {
        "w_ada": w_ada,
        "b_adaT": _colT(b_ada, 48),
        "b_ada_g": np.ascontiguousarray(np.stack([b_ada[2 * D:3 * D], b_ada[5 * D:6 * D]])),
        "n1T": _colT(inp["norm1_g"][0], 8),
        "n2T": _colT(inp["norm2_g"][0], 8),
        "w_in": f(inp["w_in"][0]),
        "b_inT": _colT(inp["b_in"][0], 40),
        "b_in_qkv": f(inp["b_in"][0][1536:3072]).reshape(1, 1536),
        "ident": np.eye(128, dtype=np.float32),
        "peer_w_q": f(inp["peer_w_q"][0]),
        "peer_keysT": np.ascontiguousarray(f(inp["peer_keys"][0]).reshape(16, 128, 128).transpose(2, 0, 1)),
        "peer_uTp": np.ascontiguousarray(f(inp["peer_u"][0]).reshape(128, 128, D).transpose(2, 1, 0)),
        "peer_v": f(inp["peer_v"][0]),
        "t_io128": np.ascontiguousarray(np.broadcast_to(np.arange(128, dtype=np.float32)[None, :], (128, 128))),
        "w_hy_out": f(inp["w_hy_out"][0]),
        "w_na_out": f(inp["w_na_out"][0]),
        "w_out": f(inp["w_out"][0]),
        "q_norm_g": f(inp["q_norm_g"][0]).reshape(1, 64),
        "k_norm_g": f(inp["k_norm_g"][0]).reshape(1, 64),
    }
    import ml_dtypes
    bf16 = ml_dtypes.bfloat16
    ar128 = np.arange(128, dtype=np.float64)
    ang = 2.0 * np.pi * np.outer(ar128, ar128) / 128.0
    Fr, Fi = np.cos(ang), -np.sin(ang)

    def fa_tab(rows):
        t = np.zeros((len(rows), 2, 2, 2, 2, 64))
        for hf_ in range(2):
            ks = slice(hf_ * 64, (hf_ + 1) * 64)
            t[:, 0, 0, hf_, 0], t[:, 0, 0, hf_, 1] = Fr[rows][:, ks], Fi[rows][:, ks]
            t[:, 0, 1, hf_, 0], t[:, 0, 1, hf_, 1] = -Fi[rows][:, ks], Fr[rows][:, ks]
        t[:, 1, :, :, 0], t[:, 1, :, :, 1] = t[:, 0, :, :, 1], t[:, 0, :, :, 0]
        return np.ascontiguousarray(t.reshape(len(rows), 2, 2, 2, 128).astype(np.float32).astype(bf16))

    def df_tab(cols):
        t = np.zeros((128, 3, len(cols)))
        t[:, 0], t[:, 1], t[:, 2] = Fr[:, cols], Fi[:, cols], -Fi[:, cols]
        return np.ascontiguousarray(t.astype(np.float32).astype(bf16))

    angt = 2.0 * np.pi * np.outer(ar128, ar128) / 16384.0
    shared["t_FAu"] = fa_tab(np.arange(128))
    shared["t_FAf"] = fa_tab(np.concatenate([np.arange(64), 127 - np.arange(64)]))
    shared["t_DFu"] = df_tab(np.arange(128))
    twr_, twi_ = np.cos(angt), -np.sin(angt)
    twt = np.zeros((128, 2, 2, 2, 64))
    for hf_ in range(2):
        ks = slice(hf_ * 64, (hf_ + 1) * 64)
        twt[:, 0, hf_, 0], twt[:, 0, hf_, 1] = twr_[:, ks], twr_[:, ks]
        twt[:, 1, hf_, 0], twt[:, 1, hf_, 1] = twi_[:, ks], twi_[:, ks]
    shared["t_twb"] = np.ascontiguousarray(twt.reshape(128, 2, 2, 128).astype(np.float32).astype(bf16))
    shared["t_sel"] = np.ascontiguousarray((np.arange(128)[:, None] % 64 == np.arange(128)[None, :] % 64).astype(np.float32))
    tl = np.linspace(0.0, 1.0, L, dtype=np.float32)[:, None]
    wl = (np.float32(2.0 * math.pi / L) * np.arange(L, dtype=np.float32))[:, None]
    bands = np.linspace(1e-4, 15.0, 16, dtype=np.float32)[None, :]
    shared["t_featsT"] = np.ascontiguousarray(np.concatenate([tl, np.cos(bands * wl), -np.sin(bands * wl)], axis=-1).T.astype(np.float32))
    deltas = np.abs(np.linspace(math.log(1e-2) / 1.5, math.log(1e-2) / 0.3, 512, dtype=np.float32))
    i512 = (np.arange(512, dtype=np.float64) / (L - 1))[None, :]
    b16 = (512.0 * np.arange(16, dtype=np.float64) / (L - 1))[None, :]
    shared["t_dec0"] = np.ascontiguousarray(np.exp(-deltas.astype(np.float64)[:, None] * i512).astype(np.float32))
    shared["t_decs"] = np.ascontiguousarray(np.exp(-deltas.astype(np.float64)[:, None] * b16).astype(np.float32))
    cw = f(inp["hy_conv_w"][0])
    shared["hy_convw"] = np.ascontiguousarray(cw.reshape(3, 12, 128).transpose(2, 1, 0))
    shared["hy_convb"] = _colT(inp["hy_conv_b"][0], 12)
    shared["hy_skipc"] = np.ascontiguousarray(f(inp["hy_skip"][0]).reshape(2, 4, 128).transpose(2, 0, 1))
    shared["hy_f1_w"] = f(inp["hy_f1_w"][0])
    shared["hy_f2_w"] = f(inp["hy_f2_w"][0])
    shared["hy_f3_w"] = f(inp["hy_f3_w"][0])
    shared["hy_f4_w"] = f(inp["hy_f4_w"][0])
    shared["hy_fbc"] = np.ascontiguousarray(np.stack([f(inp["hy_f1_b"][0]), f(inp["hy_f2_b"][0]), f(inp["hy_f3_b"][0]),
                                                      f(inp["hy_sin_freq"][0])], axis=1))
    inv = (10000.0 ** (-np.arange(16, dtype=np.float32) / np.float32(16))).astype(np.float32)
    rpb = f(inp["na_rpb"][0])
    kl = np.arange(2)[:, None, None, None, None]
    ck = np.arange(64)[None, :, None, None, None]
    cc = np.arange(8)[None, None, :, None, None]
    qr_ = np.arange(8)[None, None, None, :, None]
    cq = np.arange(64)[None, None, None, None, :]
    dr = 2 * cc + kl - qr_ + 3
    dc = np.clip(ck - cq, -15, 15) + 15
    drv = (dr >= 0) & (dr <= 14)
    bx = rpb[:, np.clip(dr, 0, 14), dc]
    bx = np.where(np.broadcast_to(drv, bx.shape[1:])[None], bx, np.float32(0.0))
    shared["biasx"] = np.ascontiguousarray(bx.reshape(8, 128, 8, 512).astype(np.float32))
    maps = []
    for core in range(N_CORES):
        b, q = core // 4, core % 4
        m = dict(shared)
        m["xroll"] = np.ascontiguousarray(np.roll(x[b], -2048 * q, axis=0))
        m["cT"] = np.ascontiguousarray(np.stack([_colT(c[b], 8), _colT(c_ctx, 8)], axis=-1))
        m["ctxb"] = np.ascontiguousarray(ctx[b])
        tiles = np.arange(64)
        hi_of = np.where(tiles < 64 - 16 * q, tiles, tiles + 64)
        m["t_FAq"] = fa_tab(hi_of)
        m["t_DFq"] = df_tab(hi_of)
        seam = (L - 2048 * q) % L
        fl = np.array([1.0 if 2048 * j == seam else 0.0 for j in range(4)], np.float32)
        omf = np.stack([1.0 - fl, 1.0 - np.roll(fl, -1)], axis=0)
        m["t_omf"] = np.ascontiguousarray(np.broadcast_to(omf[None], (128, 2, 4)).astype(np.float32))
        jj = np.arange(4)[:, None, None, None, None, None]
        rk = 32 * q + 8 * jj + 2 * cc[None] - 4 + kl[None]
        rq = 32 * q + 8 * jj + qr_[None]
        rs = np.clip(rq - 4, 0, 120)
        cs = np.clip(cq[None] - 8, 0, 48)
        vis = (rk >= rs) & (rk < rs + 8) & (rk >= 0) & (rk < 128) & (ck[None] >= cs) & (ck[None] < cs + 16)
        m["negmask"] = np.ascontiguousarray(np.where(vis, np.float32(0.0), np.float32(-30000.0)).reshape(4, 128, 8, 512).astype(np.float32))
        pos = (np.arange(20 * 128) - 256 + 2048 * q) % L
        ar = (pos // 64).astype(np.float32)[:, None] * inv[None, :]
        ac = (pos % 64).astype(np.float32)[:, None] * inv[None, :]
        m["rope"] = np.ascontiguousarray(np.concatenate([np.cos(ar), np.cos(ac), np.sin(ar), np.sin(ac)], axis=1).astype(np.float32))
        maps.append(m)
    return maps


def kernel(**inputs):
    nc, es, dbg = build_program()
    maps = prep_inputs(inputs)
    res = run_bass_kernel_spmd(nc, maps, core_ids=list(range(N_CORES)))
    es.close()
    outp = np.zeros((2, L, D), np.float32)
    for core in range(N_CORES):
        b, q = core // 4, core % 4
        outp[b, 2048 * q:2048 * (q + 1)] = res.results[core]["out"]
    return outp
```
